# Optimizing a Trainium2 kernel written in Bass

```python
import math
import jax, jax.numpy as jnp
from jax import lax
import numpy as np

D_MODEL = 1024
BATCH = 8
SEQ = 2048
DEPTH = 2

N_MIXERS = 2
HEAD_DIM = 64
GRID_W = 64
N_MEM = 256
MEM_HEADS = 4
A_Q_HEADS = 12
A_KV_HEADS = 4
A_GROUP = A_Q_HEADS // A_KV_HEADS
Q_BLOCK = 128
AXIAL_THETA = 10000.0
B_HEADS = 8
DILATED_PATTERNS = ((128, 1), (512, 4), (2048, 16))
N_PATTERNS = len(DILATED_PATTERNS)
PARTIAL_ROT_DIMS = HEAD_DIM // 4
PARTIAL_THETA = 500000.0
N_EXPERTS = 16
N_EXPERT_GROUPS = 4
EXPERTS_PER_GROUP = N_EXPERTS // N_EXPERT_GROUPS
TOP_K = 2
D_EXPERT = 512
ALPHA = (2 * DEPTH) ** 0.25
BETA = (8 * DEPTH) ** -0.25
NORM_EPS = 1e-6
NEG_BIG = -1e30
N_A_LAYERS = (DEPTH + 1) // 2
N_B_LAYERS = DEPTH // 2
A_IN = (A_Q_HEADS + 2 * A_KV_HEADS + MEM_HEADS) * HEAD_DIM
A_OUT = (A_Q_HEADS + MEM_HEADS) * HEAD_DIM
B_IN = (3 * N_PATTERNS * B_HEADS + MEM_HEADS) * HEAD_DIM
B_OUT = (B_HEADS + MEM_HEADS) * HEAD_DIM

kernel_name = 'hybrid_gqa_dilated_moe_encoder'

F32 = jnp.float32


def layer_norm(x, g, b):
    xf = x.astype(F32)
    mu = jnp.mean(xf, -1, keepdims=True)
    var = jnp.mean(jnp.square(xf - mu), -1, keepdims=True)
    return ((xf - mu) * lax.rsqrt(var + NORM_EPS) * g.astype(F32) + b.astype(F32)).astype(x.dtype)


def rms_norm(x, g):
    xf = x.astype(F32)
    return (xf * lax.rsqrt(jnp.mean(xf * xf, -1, keepdims=True) + NORM_EPS) * g.astype(F32)).astype(x.dtype)


def rope_freqs(n, theta):
    return 1.0 / (theta ** (jnp.arange(0, n, 2, dtype=F32) / n))


def rotate(x, ang):
    cos = jnp.cos(ang)[:, None, :]
    sin = jnp.sin(ang)[:, None, :]
    xf = x.astype(F32)
    x1, x2 = jnp.split(xf, 2, axis=-1)
    return jnp.concatenate([x1 * cos - x2 * sin, x2 * cos + x1 * sin], -1).astype(x.dtype)


def axial_rope(x, seq_len):
    rows = seq_len // GRID_W
    row = jnp.repeat(jnp.arange(rows), GRID_W).astype(F32)
    col = jnp.tile(jnp.arange(GRID_W), rows).astype(F32)
    half = HEAD_DIM // 2
    inv = rope_freqs(half, AXIAL_THETA)
    return jnp.concatenate([rotate(x[..., :half], row[:, None] * inv),
                            rotate(x[..., half:], col[:, None] * inv)], -1)


def partial_rope(x, seq_len):
    pos = jnp.arange(seq_len, dtype=F32)
    ang = pos[:, None] * rope_freqs(PARTIAL_ROT_DIMS, PARTIAL_THETA)
    return jnp.concatenate([rotate(x[..., :PARTIAL_ROT_DIMS], ang), x[..., PARTIAL_ROT_DIMS:]], -1)


def gqa_block_attention(q, k, v):
    b, s = q.shape[:2]
    nq = s // Q_BLOCK
    scale = HEAD_DIM ** -0.5
    qb = q.reshape(b, nq, Q_BLOCK, A_KV_HEADS, A_GROUP, HEAD_DIM).transpose(1, 0, 2, 3, 4, 5)

    def block(qblk):
        sc = jnp.einsum('bqkgd,bskd->bkgqs', qblk, k, preferred_element_type=F32) * scale
        p = jax.nn.softmax(sc, axis=-1).astype(v.dtype)
        return jnp.einsum('bkgqs,bskd->bqkgd', p, v)

    o = lax.map(block, qb)
    return o.transpose(1, 0, 2, 3, 4, 5).reshape(b, s, A_Q_HEADS * HEAD_DIM)


def dilated_band_attention(q, k, v, dilation, n_side):
    b, s, h, hd = q.shape
    L = s // dilation
    P = n_side
    nb = -(-L // P)
    Lp = nb * P
    r = b * dilation

    def to_residue(t):
        return t.reshape(b, L, dilation, h, hd).transpose(0, 2, 1, 3, 4).reshape(r, L, h, hd)

    qr, kr, vr = to_residue(q), to_residue(k), to_residue(v)
    qr = jnp.pad(qr, ((0, 0), (0, Lp - L), (0, 0), (0, 0)))
    pad_kv = ((0, 0), (P, Lp - L + P), (0, 0), (0, 0))
    kr = jnp.pad(kr, pad_kv)
    vr = jnp.pad(vr, pad_kv)

    def windows(t):
        tb = t.reshape(r, nb + 2, P, h, hd)
        return jnp.concatenate([tb[:, :-2], tb[:, 1:-1], tb[:, 2:]], axis=2)

    kw, vw = windows(kr), windows(vr)
    qb = qr.reshape(r, nb, P, h, hd)
    qpos = jnp.arange(nb)[:, None] * P + jnp.arange(P)[None, :]
    kpos = (jnp.arange(nb)[:, None] - 1) * P + jnp.arange(3 * P)[None, :]
    dist = qpos[:, :, None] - kpos[:, None, :]
    valid = (jnp.abs(dist) <= n_side) & (kpos[:, None, :] >= 0) & (kpos[:, None, :] < L)
    sc = jnp.einsum('rnqhd,rnkhd->rnhqk', qb, kw, preferred_element_type=F32) * (HEAD_DIM ** -0.5)
    sc = jnp.where(valid[None, :, None], sc, NEG_BIG)
    lse = jax.nn.logsumexp(sc, axis=-1)
    p = jnp.exp(sc - lse[..., None]).astype(v.dtype)
    o = jnp.einsum('rnhqk,rnkhd->rnqhd', p, vw).reshape(r, Lp, h, hd)[:, :L]
    o = o.reshape(b, dilation, L, h, hd).transpose(0, 2, 1, 3, 4).reshape(b, s, h, hd)
    lse = lse.transpose(0, 1, 3, 2).reshape(r, Lp, h)[:, :L]
    lse = lse.reshape(b, dilation, L, h).transpose(0, 2, 1, 3).reshape(b, s, h)
    return o, lse


def dilated_mixture(q, k, v):
    b, s = q.shape[:2]
    outs, lses = [], []
    for g, (window, dilation) in enumerate(DILATED_PATTERNS):
        o, l = dilated_band_attention(q[:, :, g], k[:, :, g], v[:, :, g], dilation, (window // 2) // dilation)
        outs.append(o)
        lses.append(l)
    wts = jax.nn.softmax(jnp.stack(lses, 0), axis=0)
    o = jnp.einsum('gbsh,gbshd->bshd', wts, jnp.stack(outs, 0).astype(F32))
    return o.astype(q.dtype).reshape(b, s, B_HEADS * HEAD_DIM)


def memory_attention(q, mk, mv):
    b, s = q.shape[:2]
    sc = jnp.einsum('bshd,bmhd->bhsm', q, mk, preferred_element_type=F32) * (HEAD_DIM ** -0.5)
    p = jax.nn.softmax(sc, axis=-1).astype(mv.dtype)
    return jnp.einsum('bhsm,bmhd->bshd', p, mv).reshape(b, s, MEM_HEADS * HEAD_DIM)


def grouped_moe(x, router_w, router_b, w_gate, w_up, w_down):
    b, s, d = x.shape
    xt = x.reshape(b * s, d)
    aff = jax.nn.sigmoid(jnp.dot(xt, router_w, preferred_element_type=F32))
    sel = aff + router_b.astype(F32)
    grp_score = lax.top_k(sel.reshape(-1, N_EXPERT_GROUPS, EXPERTS_PER_GROUP), TOP_K)[0].sum(-1)
    best = jnp.argmax(grp_score, axis=-1)
    in_grp = (jnp.arange(N_EXPERTS) // EXPERTS_PER_GROUP)[None, :] == best[:, None]
    _, idx = lax.top_k(jnp.where(in_grp, sel, -jnp.inf), TOP_K)
    w = jnp.take_along_axis(aff, idx, axis=-1)
    w = w / jnp.sum(w, -1, keepdims=True)
    gates = jnp.sum(jax.nn.one_hot(idx, N_EXPERTS, dtype=F32) * w[..., None], axis=1)

    def expert_step(acc, p):
        wg, wu, wd, g = p
        hdn = jax.nn.silu(xt @ wg) * (xt @ wu)
        return acc + g[:, None] * (hdn @ wd).astype(F32), None

    acc, _ = lax.scan(expert_step, jnp.zeros((b * s, d), F32), (w_gate, w_up, w_down, gates.T))
    return acc.astype(x.dtype).reshape(b, s, d)


def setup_inputs(seed: int = 0) -> dict:
    key = jax.random.key(seed)
    ks = jax.random.split(key, 20)
    hd = HEAD_DIM
    nrm = jax.random.normal
    x = nrm(ks[0], (BATCH, SEQ, D_MODEL), F32)
    mem = nrm(ks[1], (BATCH, N_MEM, D_MODEL), F32)
    mkv_scale = jnp.concatenate([jnp.ones((MEM_HEADS * hd,), F32), jnp.full((MEM_HEADS * hd,), BETA, F32)])
    w_mem_kv = nrm(ks[2], (D_MODEL, 2 * MEM_HEADS * hd), F32) * D_MODEL ** -0.5 * mkv_scale
    router_w = nrm(ks[3], (D_MODEL, N_EXPERTS), F32) * D_MODEL ** -0.5
    router_b = nrm(ks[4], (N_EXPERTS,), F32) * 0.01
    a_scale = jnp.concatenate([jnp.ones(((A_Q_HEADS + A_KV_HEADS) * hd,), F32),
                               jnp.full((A_KV_HEADS * hd,), BETA, F32),
                               jnp.ones((MEM_HEADS * hd,), F32)])
    a_w_in = nrm(ks[5], (N_A_LAYERS, D_MODEL, A_IN), F32) * D_MODEL ** -0.5 * a_scale
    a_w_out = nrm(ks[6], (N_A_LAYERS, A_OUT, D_MODEL), F32) * A_OUT ** -0.5 * BETA
    a_q_norm = 1.0 + 0.02 * nrm(ks[7], (N_A_LAYERS, hd), F32)
    a_k_norm = 1.0 + 0.02 * nrm(ks[8], (N_A_LAYERS, hd), F32)
    b_scale = jnp.concatenate([jnp.ones((2 * N_PATTERNS * B_HEADS * hd,), F32),
                               jnp.full((N_PATTERNS * B_HEADS * hd,), BETA, F32),
                               jnp.ones((MEM_HEADS * hd,), F32)])
    b_w_in = nrm(ks[9], (N_B_LAYERS, D_MODEL, B_IN), F32) * D_MODEL ** -0.5 * b_scale
    b_w_out = nrm(ks[10], (N_B_LAYERS, B_OUT, D_MODEL), F32) * B_OUT ** -0.5 * BETA
    ln1_g = 1.0 + 0.02 * nrm(ks[11], (DEPTH, D_MODEL), F32)
    ln1_b = 0.02 * nrm(ks[12], (DEPTH, D_MODEL), F32)
    ln2_g = 1.0 + 0.02 * nrm(ks[13], (DEPTH, D_MODEL), F32)
    ln2_b = 0.02 * nrm(ks[14], (DEPTH, D_MODEL), F32)
    w_gate = nrm(ks[15], (DEPTH, N_EXPERTS, D_MODEL, D_EXPERT), F32) * D_MODEL ** -0.5
    w_up = nrm(ks[16], (DEPTH, N_EXPERTS, D_MODEL, D_EXPERT), F32) * D_MODEL ** -0.5
    w_down = nrm(ks[17], (DEPTH, N_EXPERTS, D_EXPERT, D_MODEL), F32) * D_EXPERT ** -0.5 * BETA
    return {'x': x, 'mem': mem, 'w_mem_kv': w_mem_kv, 'router_w': router_w, 'router_b': router_b,
            'a_w_in': a_w_in, 'a_w_out': a_w_out, 'a_q_norm': a_q_norm, 'a_k_norm': a_k_norm,
            'b_w_in': b_w_in, 'b_w_out': b_w_out,
            'ln1_g': ln1_g, 'ln1_b': ln1_b, 'ln2_g': ln2_g, 'ln2_b': ln2_b,
            'w_gate': w_gate, 'w_up': w_up, 'w_down': w_down}


def reference(x, mem, w_mem_kv, router_w, router_b, a_w_in, a_w_out, a_q_norm, a_k_norm,
              b_w_in, b_w_out, ln1_g, ln1_b, ln2_g, ln2_b, w_gate, w_up, w_down):
    b, s, _ = x.shape
    hd = HEAD_DIM
    mkv = jnp.einsum('bmd,de->bme', mem, w_mem_kv)
    mem_k, mem_v = jnp.split(mkv, 2, axis=-1)
    mem_k = mem_k.reshape(b, N_MEM, MEM_HEADS, hd)
    mem_v = mem_v.reshape(b, N_MEM, MEM_HEADS, hd)
    a_split = [A_Q_HEADS * hd, (A_Q_HEADS + A_KV_HEADS) * hd, (A_Q_HEADS + 2 * A_KV_HEADS) * hd]
    nb_cols = N_PATTERNS * B_HEADS * hd
    b_split = [nb_cols, 2 * nb_cols, 3 * nb_cols]
    for i in range(DEPTH):
        j = i // N_MIXERS
        if i % N_MIXERS == 0:
            proj = x @ a_w_in[j]
            qa, ka, va, qm = jnp.split(proj, a_split, axis=-1)
            qa = axial_rope(rms_norm(qa.reshape(b, s, A_Q_HEADS, hd), a_q_norm[j]), s)
            ka = axial_rope(rms_norm(ka.reshape(b, s, A_KV_HEADS, hd), a_k_norm[j]), s)
            va = va.reshape(b, s, A_KV_HEADS, hd)
            mix = gqa_block_attention(qa, ka, va)
            w_out = a_w_out[j]
        else:
            proj = x @ b_w_in[j]
            qb, kb, vb, qm = jnp.split(proj, b_split, axis=-1)
            qb = partial_rope(qb.reshape(b, s, N_PATTERNS * B_HEADS, hd), s).reshape(b, s, N_PATTERNS, B_HEADS, hd)
            kb = partial_rope(kb.reshape(b, s, N_PATTERNS * B_HEADS, hd), s).reshape(b, s, N_PATTERNS, B_HEADS, hd)
            vb = vb.reshape(b, s, N_PATTERNS, B_HEADS, hd)
            mix = dilated_mixture(qb, kb, vb)
            w_out = b_w_out[j]
        memo = memory_attention(qm.reshape(b, s, MEM_HEADS, hd), mem_k, mem_v)
        attn = jnp.concatenate([mix, memo], axis=-1) @ w_out
        x = layer_norm(ALPHA * x + attn, ln1_g[i], ln1_b[i])
        ffn = grouped_moe(x, router_w, router_b, w_gate[i], w_up[i], w_down[i])
        x = layer_norm(ALPHA * x + ffn, ln2_g[i], ln2_b[i])
    return x
```

```python
import bisect
import math
from contextlib import ExitStack
import numpy as np
import concourse.bass as bass
import concourse.mybir as mybir
from concourse.bass_utils import run_bass_kernel_spmd

F32 = mybir.dt.float32
BF16 = mybir.dt.bfloat16
AF = mybir.ActivationFunctionType
ALU = mybir.AluOpType
AX = mybir.AxisListType

S = 2048
D = 1024
NT = 16
NE = 16
DEPTH = 2
ALPHA = (2 * DEPTH) ** 0.25
EPS = 1e-6
CAP = 384
I32 = mybir.dt.int32
ENGS = ("pe", "act", "dve", "pool", "sp")


class Buf:
    __slots__ = ("name", "w", "r")

    def __init__(self, name):
        self.name = name
        self.w = None
        self.r = []


class Ins:
    __slots__ = ("eng", "fn", "waits", "inc", "pos", "incval", "dma_owner", "region")

    def __init__(self, eng, fn):
        self.eng = eng
        self.fn = fn
        self.region = None
        self.waits = []
        self.inc = False
        self.incval = None
        self.dma_owner = None


class Prog:
    def __init__(self):
        self.streams = {e: [] for e in ENGS}
        self.inc_pos = {e: [] for e in ENGS}
        self.inc_cnt = {e: 0 for e in ENGS}
        self.waited = {e: {} for e in ENGS}
        self.dma_owners = {}
        self.region = None

    def region_begin(self, r):
        self.region = r
        self.waited = {e: {} for e in ENGS}

    def region_end(self):
        self.region = None
        self.waited = {e: {} for e in ENGS}

    def _resolve(self, tok, eng):
        if tok[0] == "c":
            ins = tok[1]
            if ins.eng == eng and eng == "pe":
                return None
            if ins.incval is None:
                lst = self.inc_pos[ins.eng]
                i = bisect.bisect_left(lst, ins.pos)
                if i < len(lst):
                    return (ins.eng, self.streams[ins.eng][lst[i]].incval)
                self.inc_cnt[ins.eng] += 1
                ins.incval = self.inc_cnt[ins.eng]
                ins.inc = True
                lst.append(ins.pos)
            return (ins.eng, ins.incval)
        return (("dma", tok[1]), tok[2])

    def _deps(self, ins, reads, writes):
        latest = {}
        need = {}

        def add(tok):
            if tok is None:
                return
            if tok[0] == "c":
                p = tok[1]
                if p.eng == ins.eng and ins.eng == "pe":
                    return
                cur = latest.get(p.eng)
                if cur is None or cur.pos < p.pos:
                    latest[p.eng] = p
            else:
                key = ("dma", tok[1])
                if need.get(key, 0) < tok[2]:
                    need[key] = tok[2]

        for b in reads:
            add(b.w)
        for b in writes:
            add(b.w)
            for t in b.r:
                add(t)
        for p in latest.values():
            r = self._resolve(("c", p), ins.eng)
            if r is not None:
                need[r[0]] = max(need.get(r[0], 0), r[1])
        w = self.waited[ins.eng]
        for key, val in need.items():
            if w.get(key, 0) >= val:
                continue
            w[key] = val
            ins.waits.append((key, val))

    def _commit(self, tok, reads, writes):
        for b in reads:
            b.r.append(tok)
        for b in writes:
            b.w = tok
            b.r = []

    def op(self, eng, fn, reads=(), writes=()):
        ins = Ins(eng, fn)
        ins.region = self.region
        ins.pos = len(self.streams[eng])
        self._deps(ins, reads, writes)
        self.streams[eng].append(ins)
        self._commit(("c", ins), reads, writes)
        return ins

    def dma(self, eng, fn, reads=(), writes=(), owner=None):
        ins = Ins(eng, fn)
        ins.region = self.region
        ins.pos = len(self.streams[eng])
        self._deps(ins, reads, writes)
        okey = owner + "@" + eng
        cnt = self.dma_owners.get(okey, 0) + 1
        self.dma_owners[okey] = cnt
        ins.dma_owner = okey
        self.streams[eng].append(ins)
        self._commit(("d", okey, 16 * cnt), reads, writes)
        return ins

    def handoff(self, olds, news):
        toks = []
        for b in olds:
            if b.w is not None:
                toks.append(b.w)
            toks.extend(b.r)
        for b in news:
            b.w = None
            b.r = list(toks)

    def finish(self):
        ins = Ins("sp", None)
        ins.pos = len(self.streams["sp"])
        for name, cnt in self.dma_owners.items():
            key = ("dma", name)
            if self.waited["sp"].get(key, 0) < 16 * cnt:
                self.waited["sp"][key] = 16 * cnt
                ins.waits.append((key, 16 * cnt))
        self.streams["sp"].append(ins)

    def emit(self, nc, stack, regs):
        sems = {}
        for e in ("pe", "act", "dve", "pool"):
            sems[e] = stack.enter_context(nc.semaphore("sem_" + e))
        for name in self.dma_owners:
            sems[("dma", name)] = stack.enter_context(nc.semaphore("sd_" + name.replace("@", "_")))
        block = stack.enter_context(nc.Block())
        engmap = {"pe": block.tensor, "act": block.scalar, "dve": block.vector,
                  "pool": block.gpsimd, "sp": block.sync}
        for e in ENGS:
            stream = self.streams[e]

            def body(engine, stream=stream, e=e):
                done = {}

                def emit_ins(ins):
                    for key, val in ins.waits:
                        engine.wait_ge(sems[key], val)
                    if ins.fn is None:
                        return
                    bi = ins.fn(engine)
                    if ins.dma_owner is not None:
                        bi.then_inc(sems[("dma", ins.dma_owner)], 16)
                    elif ins.inc:
                        bi.then_inc(sems[e], 1)

                def account(ins, d):
                    if ins.dma_owner is not None:
                        k = ("dma", ins.dma_owner)
                        d[k] = d.get(k, 0) + 16
                    elif ins.inc:
                        d[e] = d.get(e, 0) + 1

                i = 0
                n = len(stream)
                while i < n:
                    r = stream[i].region
                    j = i
                    while j < n and stream[j].region == r:
                        j += 1
                    group = stream[i:j]
                    if r is None:
                        for ins in group:
                            emit_ins(ins)
                            account(ins, done)
                    else:
                        comp = {}
                        for ins in group:
                            account(ins, comp)
                        guard = engine.If_eq(regs[e], 0) if r[0] == "R" else engine.If_ne(regs[e], 0)
                        with guard:
                            for ins in group:
                                emit_ins(ins)
                        with engine.Else():
                            for k, amt in comp.items():
                                if done.get(k, 0) > 0:
                                    engine.wait_ge(sems[k], done[k])
                                engine.sem_inc(sems[k], amt)
                        for k, amt in comp.items():
                            done[k] = done.get(k, 0) + amt
                    i = j

            engmap[e](body)


def build(debug_stop=None):
    nc = bass.Bass("TRN2", target_bir_lowering=False)

    def din(name, shape):
        return nc.dram_tensor(name, list(shape), F32, kind="ExternalInput")

    x_h = din("x", [S, D]); mem_h = din("mem", [256, D]); wkv_h = din("wkv", [D, 512])
    rw_h = din("rw", [D, 16]); rb_h = din("rb", [1, 16])
    awin_h = din("awin", [D, 1536]); awout_h = din("awout", [1024, D])
    aqn_h = din("aqn", [1, 64]); akn_h = din("akn", [1, 64])
    bwin_h = din("bwin", [D, 4864]); bwout_h = din("bwout", [768, D])
    lnv_h = din("lnv", [8, D])
    wg_h = din("wg", [2, NE, D, 512]); wu_h = din("wu", [2, NE, D, 512]); wd_h = din("wd", [2, NE, 512, D])
    cosA_h = din("cosA", [S, 64]); sinA_h = din("sinA", [S, 64]); tabB_h = din("tabB", [3, S, 32])
    mask_h = din("masks", [128, 384])
    swp_h = din("swp", [128, 128])
    ltri_h = din("ltri", [128, 128]); ecap_h = din("ecap", [1, 16])
    NSLOT = NE * CAP
    xs_h = nc.dram_tensor("xs_scr", [NSLOT, D], BF16)
    ys_h = nc.dram_tensor("ys_scr", [NSLOT, D], F32)
    wbg_h = nc.dram_tensor("wbg_scr", [2, NE, D, 512], BF16)
    wbu_h = nc.dram_tensor("wbu_scr", [2, NE, D, 512], BF16)
    wbd_h = nc.dram_tensor("wbd_scr", [2, NE, 512, D], BF16)
    y_h = nc.dram_tensor("y", [S, D], F32, kind="ExternalOutput")

    P = Prog()
    st = ExitStack()
    with st:
        X = st.enter_context(nc.sbuf_tensor("X", [128, NT, D], F32))
        XT = st.enter_context(nc.sbuf_tensor("XT", [128, 8, S], BF16))
        AR = st.enter_context(nc.sbuf_tensor("AR", [128, 36864], BF16))
        FA = st.enter_context(nc.sbuf_tensor("FA", [128, 4096], F32))
        ident = st.enter_context(nc.sbuf_tensor("ident", [128, 128], BF16))
        xbf = st.enter_context(nc.sbuf_tensor("xbf", [128, 2, D], BF16))
        PT = st.enter_context(nc.sbuf_tensor("PT", [128, 3, 512], BF16))
        memKT = st.enter_context(nc.sbuf_tensor("memKT", [128, 2, 256], BF16))
        memV = st.enter_context(nc.sbuf_tensor("memV", [128, 2, 4, 128], BF16))
        rwb = st.enter_context(nc.sbuf_tensor("rwb", [128, 8, 16], BF16))
        rbt = st.enter_context(nc.sbuf_tensor("rbt", [128, 16], F32))
        maskb = st.enter_context(nc.sbuf_tensor("maskb", [128, 384], BF16))
        swpf = st.enter_context(nc.sbuf_tensor("swpf", [128, 128], F32))
        SLF = st.enter_context(nc.sbuf_tensor("SLF", [128, 4, 16], F32))
        SLI = st.enter_context(nc.sbuf_tensor("SLI", [128, 2, 16], I32))
        fi = st.enter_context(nc.sbuf_tensor("fi", [128, 2], I32))
        maskbf = st.enter_context(nc.sbuf_tensor("maskbf", [128, 256], BF16))
        ltri = st.enter_context(nc.sbuf_tensor("ltri_sb", [128, 128], BF16))
        onesm = st.enter_context(nc.sbuf_tensor("onesm", [128, 128], BF16))
        ecap = st.enter_context(nc.sbuf_tensor("ecap_sb", [128, 16], F32))
        regs = {"pe": st.enter_context(nc.tensor.register("flag_pe")), "act": st.enter_context(nc.scalar.register("flag_act")),
                "dve": st.enter_context(nc.vector.register("flag_dve")), "pool": st.enter_context(nc.gpsimd.register("flag_pool")),
                "sp": st.enter_context(nc.sync.register("flag_sp"))}
        stt_ = st.enter_context(nc.sbuf_tensor("stt", [128, 4, 16], F32))
        GATES = st.enter_context(nc.sbuf_tensor("GATES", [128, 256], F32))
        RT = st.enter_context(nc.sbuf_tensor("RT", [128, 6, 256], F32))
        tb16 = st.enter_context(nc.sbuf_tensor("tb16", [128, 2, 32], F32))
        cs = st.enter_context(nc.sbuf_tensor("cs", [128, 2, 128], F32))
        ps_s = [st.enter_context(nc.psum_tensor("ps_s%d" % i, [128, 512], F32)) for i in range(2)]
        ps_a = [st.enter_context(nc.psum_tensor("ps_a%d" % i, [128, 512], F32)) for i in range(2)]
        ps_p = [st.enter_context(nc.psum_tensor("ps_p%d" % i, [128, 512], F32)) for i in range(2)]
        ps_t = st.enter_context(nc.psum_tensor("ps_t", [128, 1024], BF16))
        ps_m = st.enter_context(nc.psum_tensor("ps_m", [128, 512], F32))

        Bx = [Buf("x%d" % i) for i in range(NT)]
        BxT = [Buf("xT%d" % i) for i in range(NT)]
        Bxbf = [Buf("xbf%d" % i) for i in range(2)]
        BPT = [Buf("PT%d" % i) for i in range(3)]
        Bps_s = [Buf("ps_s%d" % i) for i in range(2)]
        Bps_a = [Buf("ps_a%d" % i) for i in range(2)]
        Bps_p = [Buf("ps_p%d" % i) for i in range(2)]
        Bps_t = Buf("ps_t"); Bps_m = Buf("ps_m")
        Bident = Buf("ident"); BmemKT = Buf("memKT"); BmemV = Buf("memV"); Brw = Buf("rw"); Brb = Buf("rb")
        Bmask = Buf("mask"); Bst = [Buf("st%d" % i) for i in range(4)]; Bgates = Buf("gates"); BRT = Buf("RT")
        Btb = [Buf("tb%d" % i) for i in range(2)]; Bcs = [Buf("cs%d" % i) for i in range(2)]
        Bydram = Buf("ydram")
        live_AR = []
        live_FA = []

        def new_bufs(live, names):
            nb = [Buf(n) for n in names]
            P.handoff(live, nb)
            return nb

        def mm(out, lhsT, rhs, start, stop, rd, wr):
            P.op("pe", lambda e: e.matmul(out, lhsT=lhsT, rhs=rhs, start=start, stop=stop), rd, wr)

        def tr(out, in_, rd, wr):
            P.op("pe", lambda e: e.transpose(out=out, in_=in_, identity=ident[:]), list(rd) + [Bident], wr)

        def act(out, in_, func, rd, wr, scale=None, bias=None):
            kw = {}
            if scale is not None:
                kw["scale"] = scale
            if bias is not None:
                kw["bias"] = bias
            P.op("act", lambda e: e.activation(out=out, in_=in_, func=func, **kw), rd, wr)

        def tt(out, in0, in1, op, rd, wr, eng="dve"):
            P.op(eng, lambda e: e.tensor_tensor(out=out, in0=in0, in1=in1, op=op), rd, wr)

        def ts(out, in0, s1, s2, op0, op1, rd, wr, eng="dve"):
            if op1 is None:
                P.op(eng, lambda e: e.tensor_scalar(out=out, in0=in0, scalar1=s1, scalar2=None, op0=op0), rd, wr)
            else:
                P.op(eng, lambda e: e.tensor_scalar(out=out, in0=in0, scalar1=s1, scalar2=s2, op0=op0, op1=op1), rd, wr)

        def stt(out, in0, scalar, in1, op0, op1, rd, wr, eng="dve"):
            P.op(eng, lambda e: e.scalar_tensor_tensor(out=out, in0=in0, scalar=scalar, in1=in1, op0=op0, op1=op1), rd, wr)

        def cp(out, in_, rd, wr, eng="dve"):
            if eng == "act":
                P.op("act", lambda e: e.copy(out=out, in_=in_), rd, wr)
            else:
                P.op(eng, lambda e: e.tensor_copy(out=out, in_=in_), rd, wr)

        def red(out, in_, op, rd, wr):
            P.op("dve", lambda e: e.tensor_reduce(out=out, in_=in_, axis=AX.X, op=op), rd, wr)

        def recip(out, in_, rd, wr):
            P.op("dve", lambda e: e.reciprocal(out=out, in_=in_), rd, wr)

        def memset(out, val, wr, eng="dve"):
            P.op(eng, lambda e: e.memset(out, val), (), wr)

        def dma(eng, out, in_, rd, wr, owner):
            P.dma(eng, lambda e: e.dma_start(out=out, in_=in_), rd, wr, owner)

        cnt = {"s": 0, "a": 0, "p": 0, "pt": 0, "xbf": 0, "st": 0, "ys": 0, "yb": 0}

        def nxt(k, n):
            v = cnt[k] % n
            cnt[k] += 1
            return v

        memset(ident[:], 0.0, [Bident], eng="pool")
        P.op("pool", lambda e: e.affine_select(out=ident[:], in_=ident[:], pattern=[[-1, 128]], compare_op=ALU.not_equal,
                                               fill=1.0, base=0, channel_multiplier=1), [Bident], [Bident])
        memset(memV[:], 1.0, [BmemV], eng="pool")
        dma("pool", rwb[:], rw_h.ap().rearrange("(c p) f -> p c f", p=128), [], [Brw], "rw")
        dma("sp", rbt[:], bass.AP(rb_h, 0, [[0, 128], [1, 16]]), [], [Brb], "rb")
        Bswp = Buf("swp")
        dma("sp", swpf[:], swp_h.ap(), [], [Bswp], "swp")
        Bltri = Buf("ltri"); Bones = Buf("onesm"); Becap = Buf("ecap"); Bzt = Buf("zt"); BXS = Buf("XS"); BYS = Buf("YS")
        Bslf = Buf("SLF"); Bsli = Buf("SLI"); Bfi = [Buf("fi0"), Buf("fi1")]; Bmaskbf = Buf("maskbf")
        memset(onesm[:], 1.0, [Bones])
        dma("sp", ecap[:], bass.AP(ecap_h, 0, [[0, 128], [1, 16]]), [], [Becap], "ecap")
        BWB = [Buf("wb0"), Buf("wb1")]

        def precast_weights(li, lo=0, hi=3 * NE):
            jobs = [(e_, src, dst) for e_ in range(NE) for src, dst in ((wg_h, wbg_h), (wu_h, wbu_h), (wd_h, wbd_h))]
            for e_, src, dst in jobs[lo:hi]:
                dma("pool", dst.ap()[li, e_], src.ap()[li, e_], [], [BWB[li]], "wb%d" % li)

        def zero_fill():
            for hh, BB, nm in ((xs_h, BXS, "zx"), (ys_h, BYS, "zy")):
                v = hh.ap().rearrange("(j p) f -> p j f", p=128)
                nj = NSLOT // 128
                for j0 in range(0, nj, 6):
                    if hh is xs_h:
                        zs = FA[:, 1024:1536].bitcast(BF16)
                        src = bass.AP(zs.tensor, zs.offset, [[zs.ap[0][0], 128], [0, 6], [1, 1024]])
                    else:
                        src = bass.AP(FA, 1024, [[4096, 128], [0, 6], [1, 1024]])
                    dma("sp", v[:, j0:j0 + 6, :], src, [BS1, BS2], [BB], nm)

        BFA0 = new_bufs(live_FA, ["fa_init"])
        live_FA = BFA0
        dma("sp", FA[:, 0:384], mask_h.ap(), [], BFA0, "fa")
        cp(maskb[:], FA[:, 0:384], BFA0, [Bmask])
        dma("sp", FA[:, 512:640], ltri_h.ap(), [], BFA0, "fa")
        cp(ltri[:], FA[:, 512:640], BFA0, [Bltri])

        def make_xT(i, cast_eng="act"):
            k = nxt("xbf", 2)
            cp(xbf[:, k, :], X[:, i, :], [Bx[i]], [Bxbf[k]], eng=cast_eng)
            for c in range(8):
                tr(ps_t[:, c * 128:(c + 1) * 128], xbf[:, k, c * 128:(c + 1) * 128], [Bxbf[k]], [Bps_t])
            cp(XT[:, :, i * 128:(i + 1) * 128], ps_t[:].rearrange("p (c t) -> p c t", c=8), [Bps_t], [BxT[i], Bps_t])

        for i in range(NT):
            dma("sp", X[:, i, :], x_h.ap()[i * 128:(i + 1) * 128, :], [], [Bx[i]], "x%d" % i)
        for i in range(NT):
            make_xT(i)

        ARv = AR[:]
        BA = new_bufs(live_AR, ["memT", "wkv"])
        live_AR = BA
        memT = ARv[:, 0:2048].rearrange("p (c t) -> p c t", c=8)
        wkv = ARv[:, 2048:6144].rearrange("p (c f) -> p c f", c=8)
        dma("pool", wkv, wkv_h.ap().rearrange("(c p) f -> p c f", p=128), [], [BA[1]], "wkv")
        BF_ = new_bufs(live_FA, ["fa_mem"])
        live_FA = BF_
        for mt in range(2):
            dma("sp", FA[:, 0:1024], mem_h.ap()[mt * 128:(mt + 1) * 128, :], [], BF_, "fa")
            k = nxt("xbf", 2)
            cp(xbf[:, k, :], FA[:, 0:1024], BF_, [Bxbf[k]], eng="act")
            for c in range(8):
                tr(ps_t[:, c * 128:(c + 1) * 128], xbf[:, k, c * 128:(c + 1) * 128], [Bxbf[k]], [Bps_t])
            cp(memT[:, :, mt * 128:(mt + 1) * 128], ps_t[:].rearrange("p (c t) -> p c t", c=8), [Bps_t], [BA[0], Bps_t])
        for mt in range(2):
            j = nxt("p", 2)
            for c in range(8):
                mm(ps_p[j][:, :], memT[:, c, mt * 128:(mt + 1) * 128], wkv[:, c, :], c == 0, c == 7, [BA[0], BA[1]], [Bps_p[j]])
            k = nxt("xbf", 2)
            cp(xbf[:, k, 0:256], ps_p[j][:, 0:256], [Bps_p[j]], [Bxbf[k], Bps_p[j]], eng="act")
            for pr in range(2):
                tr(ps_t[:, pr * 128:(pr + 1) * 128], xbf[:, k, pr * 128:(pr + 1) * 128], [Bxbf[k]], [Bps_t])
            cp(memKT[:, :, mt * 128:(mt + 1) * 128], ps_t[:, 0:256].rearrange("p (c t) -> p c t", c=2), [Bps_t], [BmemKT, Bps_t])
            cp(memV[:, mt, 0:2, 0:64], ps_p[j][:, 256:384].rearrange("p (h d) -> p h d", h=2), [Bps_p[j]], [BmemV, Bps_p[j]])
            cp(memV[:, mt, 2:4, 64:128], ps_p[j][:, 384:512].rearrange("p (h d) -> p h d", h=2), [Bps_p[j]], [BmemV, Bps_p[j]])

        def run_attn(heads, rec_ap, Brec, ring, look, accs=None):
            if accs is None:
                accs = [(ps_a[0][:, :], Bps_a[0]), (ps_a[1][:, :], Bps_a[1])]
            steps = []
            for h0 in range(0, len(heads), 2):
                for kt in range(heads[h0]["nkt"]):
                    steps.append((h0, kt))
                    steps.append((h0 + 1, kt))
            sbuf_of = {}
            rc = [0]

            def issue_qk(idx):
                hi, kt = steps[idx]
                h = heads[hi]
                t_, b_ = ring[rc[0] % len(ring)]
                rc[0] += 1
                sbuf_of[idx] = (t_, b_)
                mm(t_[:, :], h["k_fn"](kt), h["q_ap"], True, True, list(h["rd_q"]) + list(h["rd_k"]), [b_])

            issue_qk(0)
            issue_qk(1)
            acc_of = {}
            ac = [0]
            for idx, (hi, kt) in enumerate(steps):
                h = heads[hi]
                if idx % 2 == 0:
                    for q in (2, 3):
                        if idx + q < len(steps):
                            issue_qk(idx + q)
                if kt == 0:
                    acc_of[hi] = accs[ac[0] % len(accs)]
                    ac[0] += 1
                a_, ab_ = acc_of[hi]
                t_, b_ = sbuf_of.pop(idx)
                jp = nxt("pt", 3)
                act(PT[:, jp, :], t_[:, :], AF.Exp, [b_], [BPT[jp], b_], scale=0.125)
                mm(a_, h["v_fn"](kt), PT[:, jp, :], kt == 0, kt == h["nkt"] - 1, [BPT[jp]] + list(h["rd_v"]), [ab_])
                if kt == h["nkt"] - 1:
                    rows, orow = h["rows"], h["orow"]
                    recip(rec_ap[orow, :], a_[orow, :], [ab_], [Brec, ab_])
                    tt(h["out_ap"], a_[rows, :], rec_ap[orow, :], ALU.mult, [ab_, Brec], list(h["wr_out"]) + [ab_])

        def layer_norm_tile(i, lng, lnb, sc, Bln, Bsc):
            k = nxt("st", 4)
            s = stt_[:, k, :]
            B_ = Bst[k]
            red(s[:, 0:1], X[:, i, :], ALU.add, [Bx[i]], [B_])
            act(sc, X[:, i, :], AF.Square, [Bx[i]], [Bsc])
            red(s[:, 1:2], sc, ALU.add, [Bsc], [B_])
            ts(s[:, 2:3], s[:, 0:1], 1.0 / D, None, ALU.mult, None, [B_], [B_])
            tt(s[:, 3:4], s[:, 2:3], s[:, 2:3], ALU.mult, [B_], [B_])
            stt(s[:, 4:5], s[:, 1:2], 1.0 / D, s[:, 3:4], ALU.mult, ALU.subtract, [B_], [B_])
            ts(s[:, 4:5], s[:, 4:5], EPS, None, ALU.add, None, [B_], [B_])
            act(s[:, 5:6], s[:, 4:5], AF.Sqrt, [B_], [B_])
            recip(s[:, 6:7], s[:, 5:6], [B_], [B_])
            stt(s[:, 7:8], s[:, 2:3], -1.0, s[:, 6:7], ALU.mult, ALU.mult, [B_], [B_])
            act(sc, X[:, i, :], AF.Identity, [Bx[i], B_], [Bsc], scale=s[:, 6:7], bias=s[:, 7:8])
            tt(sc, sc, lng, ALU.mult, [Bsc, Bln], [Bsc])
            tt(X[:, i, :], sc, lnb, ALU.add, [Bsc, Bln], [Bx[i]])

        def ln_batched(prep_fn, post_fn, lng, lnb, Bln):
            nonlocal live_FA
            Bsc = new_bufs(ln_prev_FA[0], ["lnsc0", "lnsc1"])
            live_FA = live_FA + Bsc
            scs = [FA[:, 2048:3072], FA[:, 3072:4096]]
            s = stt_[:]
            for i in range(NT):
                prep_fn(i)
                k = i % 2
                red(s[:, 0, i:i + 1], X[:, i, :], ALU.add, [Bx[i]], Bst)
                P.op("act", lambda e, i=i, k=k: e.activation(out=scs[k], in_=X[:, i, :], func=AF.Square, accum_out=s[:, 1, i:i + 1]),
                     [Bx[i]], [Bsc[k]] + Bst)
            ts(s[:, 0, :], s[:, 0, :], 1.0 / D, None, ALU.mult, None, Bst, Bst)
            tt(s[:, 2, :], s[:, 0, :], s[:, 0, :], ALU.mult, Bst, Bst)
            stt(s[:, 1, :], s[:, 1, :], 1.0 / D, s[:, 2, :], ALU.mult, ALU.subtract, Bst, Bst)
            ts(s[:, 1, :], s[:, 1, :], EPS, None, ALU.add, None, Bst, Bst)
            act(s[:, 1, :], s[:, 1, :], AF.Sqrt, Bst, Bst)
            recip(s[:, 1, :], s[:, 1, :], Bst, Bst)
            stt(s[:, 3, :], s[:, 0, :], -1.0, s[:, 1, :], ALU.mult, ALU.mult, Bst, Bst)
            for i in range(NT):
                k = i % 2
                act(scs[k], X[:, i, :], AF.Identity, [Bx[i]] + Bst, [Bsc[k]], scale=s[:, 1, i:i + 1], bias=s[:, 3, i:i + 1])
                tt(scs[k], scs[k], lng, ALU.mult, [Bsc[k], Bln], [Bsc[k]], eng="pool")
                tt(X[:, i, :], scs[k], lnb, ALU.add, [Bsc[k], Bln], [Bx[i]])
                post_fn(i)

        ln_prev_FA = [[]]

        def load_ln(li, which):
            nonlocal live_FA
            ln_prev_FA[0] = list(live_FA)
            Bn = new_bufs(live_FA, ["ln", "lnsc"])
            live_FA = Bn
            r0 = li * 4 + which * 2
            dma("sp", FA[:, 0:1024], bass.AP(lnv_h, r0 * D, [[0, 128], [1, D]]), [], [Bn[0]], "fa")
            dma("sp", FA[:, 1024:2048], bass.AP(lnv_h, (r0 + 1) * D, [[0, 128], [1, D]]), [], [Bn[0]], "fa")
            return FA[:, 0:1024], FA[:, 1024:2048], FA[:, 2048:3072], Bn[0], Bn[1]

        def out_proj_ln(li, nch, wout_h, OT_fn, B_OT, last_for_next):
            nonlocal live_AR
            Bw = new_bufs([], ["wout"])
            wo = ARv[:, 28672:28672 + nch * 1024].rearrange("p (c f) -> p c f", c=nch)
            P.handoff(live_AR + live_AR_tail[0], Bw)
            live_AR_tail[0] = Bw
            dma("pool", wo, wout_h.ap().rearrange("(c p) f -> p c f", p=128), [], Bw, "wout")
            lng, lnb, sc, Bln, Bsc = load_ln(li, 0)
            def opA(i):
                for half in range(2):
                    j = nxt("p", 2)
                    for p in range(nch):
                        mm(ps_p[j][:, :], OT_fn(p, i), wo[:, p, half * 512:(half + 1) * 512], p == 0, p == nch - 1,
                           list(B_OT(p, i)) + Bw, [Bps_p[j]])
                    stt(X[:, i, half * 512:(half + 1) * 512], X[:, i, half * 512:(half + 1) * 512], ALPHA, ps_p[j][:, :],
                        ALU.mult, ALU.add, [Bx[i], Bps_p[j]], [Bx[i], Bps_p[j]])

            ln_batched(opA, make_xT, lng, lnb, Bln)

        live_AR_tail = [[]]

        def moe(li):
            nonlocal live_AR, live_FA
            for i in range(NT):
                for c in range(8):
                    mm(ps_m[:, i * 16:(i + 1) * 16], XT[:, c, i * 128:(i + 1) * 128], rwb[:, c, :], c == 0, c == 7,
                       [BxT[i], Brw], [Bps_m])
            AFF = RT[:, 0, :]; SEL = RT[:, 1, :]; T2 = RT[:, 2, :]; T3 = RT[:, 3, :]; SM = RT[:, 4, :]
            M1 = SM[:, 0:64]; M2 = SM[:, 64:128]; GS = SM[:, 128:192]; GM = SM[:, 192:208]; DEN = SM[:, 208:224]
            RB_ = [BRT]
            act(AFF, ps_m[:, 0:256], AF.Sigmoid, [Bps_m], [BRT, Bps_m])
            tt(SEL.rearrange("p (t e) -> p t e", e=16), AFF.rearrange("p (t e) -> p t e", e=16),
               bass.AP(rbt, 0, [[16, 128], [0, 16], [1, 16]]), ALU.add, [BRT, Brb], RB_)
            sel3 = SEL.rearrange("p (g e) -> p g e", e=4)
            red(M1, sel3, ALU.max, RB_, RB_)
            tt(T2.rearrange("p (g e) -> p g e", e=4), sel3, M1.to_broadcast([128, 64, 4]), ALU.is_equal, RB_, RB_)
            stt(T3, T2, -1.0e9, SEL, ALU.mult, ALU.add, RB_, RB_)
            red(M2, T3.rearrange("p (g e) -> p g e", e=4), ALU.max, RB_, RB_)
            tt(GS, M1, M2, ALU.add, RB_, RB_)
            red(GM, GS.rearrange("p (t g) -> p t g", g=4), ALU.max, RB_, RB_)
            tt(GS.rearrange("p (t g) -> p t g", g=4), GS.rearrange("p (t g) -> p t g", g=4), GM.to_broadcast([128, 16, 4]),
               ALU.is_equal, RB_, RB_)
            tt(T2.rearrange("p (g e) -> p g e", e=4), sel3, M2.to_broadcast([128, 64, 4]), ALU.is_ge, RB_, RB_)
            tt(T2.rearrange("p (g e) -> p g e", e=4), T2.rearrange("p (g e) -> p g e", e=4), GS.to_broadcast([128, 64, 4]),
               ALU.mult, RB_, RB_)
            tt(T3, T2, AFF, ALU.mult, RB_, RB_)
            red(DEN, T3.rearrange("p (t e) -> p t e", e=16), ALU.add, RB_, RB_)
            recip(DEN, DEN, RB_, RB_)
            tt(GATES[:].rearrange("p (t e) -> p t e", e=16), T3.rearrange("p (t e) -> p t e", e=16),
               DEN.to_broadcast([128, 16, 16]), ALU.mult, RB_, [Bgates])
            BRT2 = new_bufs(live_FA, ["RT2"])[0]
            live_FA = [BRT2]
            RT2 = FA[:, 0:2048].rearrange("p (a b) -> p a b", a=8)
            R2_ = [BRT2]
            MASK = T2
            cp(maskbf[:], MASK, RB_, [Bmaskbf])
            mm(ps_m[:, 0:256], ltri[:], maskbf[:], True, True, [Bltri, Bmaskbf], [Bps_m])
            mm(ps_m[:, 256:512], onesm[:], maskbf[:], True, True, [Bones, Bmaskbf], [Bps_m])
            PRE = RT2[:, 0, :]; ISZ = RT2[:, 1, :]; FIRST = RT2[:, 2, :]; SECOND = RT2[:, 3, :]; POS = RT2[:, 4, :]
            TMP = RT2[:, 5, :]; OFFS = RT2[:, 6, :]; TTs = RT2[:, 7, :]
            cp(POS, ps_m[:, 0:256], [Bps_m], R2_ + [Bps_m])
            cp(TTs, ps_m[:, 256:512], [Bps_m], R2_ + [Bps_m])
            offs3 = OFFS.rearrange("p (t e) -> p t e", e=16)
            tts3 = TTs.rearrange("p (t e) -> p t e", e=16)
            memset(offs3[:, 0, :], 0.0, R2_)
            for i in range(1, NT):
                tt(offs3[:, i, :], offs3[:, i - 1, :], tts3[:, i - 1, :], ALU.add, R2_, R2_)
            TOTE = RT[:, 5, 0:16]
            tt(TOTE, offs3[:, NT - 1, :], tts3[:, NT - 1, :], ALU.add, R2_, RB_)
            red(RT[:, 5, 16:17], TOTE, ALU.max, RB_, RB_)
            ts(RT[:, 5, 17:18], RT[:, 5, 16:17], float(CAP) + 0.5, None, ALU.is_gt, None, RB_, RB_)
            cp(fi[:, li:li + 1], RT[:, 5, 17:18], RB_, [Bfi[li]])
            for e_ in ENGS:
                P.op(e_, lambda e, r=regs[e_]: e.reg_load(r, fi[0:1, li:li + 1]), [Bfi[li]], [])
            tt(POS, POS, OFFS, ALU.add, R2_, R2_)
            tt(POS.rearrange("p (t e) -> p t e", e=16), POS.rearrange("p (t e) -> p t e", e=16),
               bass.AP(ecap, 0, [[16, 128], [0, 16], [1, 16]]), ALU.add, R2_ + [Becap], R2_)
            m3 = MASK.rearrange("p (g e) -> p g e", e=4)
            pre3 = PRE.rearrange("p (g e) -> p g e", e=4)
            memset(PRE, 0.0, R2_)
            cp(pre3[:, :, 1], m3[:, :, 0], RB_ + R2_, R2_)
            tt(pre3[:, :, 2], m3[:, :, 0], m3[:, :, 1], ALU.add, RB_ + R2_, R2_)
            tt(pre3[:, :, 3], pre3[:, :, 2], m3[:, :, 2], ALU.add, RB_ + R2_, R2_)
            ts(ISZ, PRE, 0.0, None, ALU.is_equal, None, R2_, R2_)
            tt(FIRST, MASK, ISZ, ALU.mult, RB_ + R2_, R2_)
            tt(SECOND, MASK, FIRST, ALU.subtract, RB_ + R2_, R2_)
            for k_, (sel_, val_, bufs_) in enumerate(((FIRST, POS, R2_), (SECOND, POS, R2_), (FIRST, GATES[:], R2_ + [Bgates]),
                                                      (SECOND, GATES[:], R2_ + [Bgates]))):
                tt(TMP, sel_, val_, ALU.mult, bufs_, R2_)
                red(SLF[:, k_, :], TMP.rearrange("p (t e) -> p t e", e=16), ALU.add, R2_, [Bslf])
            cp(SLI[:], SLF[:, 0:2, :], [Bslf], [Bsli])

            P.region_begin(("R", li))
            names = []
            for s_ in range(2):
                names += ["rwg%d" % s_, "rwu%d" % s_, "rwd%d" % s_]
            names += ["xstm0", "xstm1", "xsT0", "xsT1"]
            Bw = new_bufs(live_AR + live_AR_tail[0], names)
            live_AR = Bw
            live_AR_tail[0] = []

            def load_expert_w(e_):
                s_ = e_ % 2
                base = s_ * 12288
                Wg = ARv[:, base:base + 4096].rearrange("p (c f) -> p c f", c=8)
                Wu = ARv[:, base + 4096:base + 8192].rearrange("p (c f) -> p c f", c=8)
                Wd = ARv[:, base + 8192:base + 12288].rearrange("p (c f) -> p c f", c=4)
                dma("sp", Wg, wbg_h.ap()[li, e_].rearrange("(c p) f -> p c f", p=128), [BWB[li]], [Bw[3 * s_]], "rwg%d" % s_)
                dma("sp", Wu, wbu_h.ap()[li, e_].rearrange("(c p) f -> p c f", p=128), [BWB[li]], [Bw[3 * s_ + 1]], "rwu%d" % s_)
                dma("sp", Wd, wbd_h.ap()[li, e_].rearrange("(c p) f -> p c f", p=128), [BWB[li]], [Bw[3 * s_ + 2]], "rwd%d" % s_)

            load_expert_w(0)
            load_expert_w(1)
            Bxb = new_bufs(live_FA, ["xb%d" % q for q in range(8)])
            live_FA = Bxb
            xb16 = FA[:].bitcast(BF16).rearrange("p (k f) -> p k f", k=8)
            for i in range(NT):
                kq = i % 8
                cp(xb16[:, kq, :], X[:, i, :], [Bx[i]], [Bxb[kq]], eng=("act" if i % 2 == 0 else "dve"))
                for k_ in range(2):
                    P.dma("pool", lambda e, i=i, k_=k_, kq=kq: e.indirect_dma_start(
                        out=xs_h.ap(), out_offset=bass.IndirectOffsetOnAxis(ap=SLI[:, k_, i:i + 1], axis=0),
                        in_=xb16[:, kq, :], in_offset=None),
                        [Bxb[kq], Bsli], [BXS], "rsc%d" % kq)
            P.region_end()
            for i in range(NT):
                P.op("act", lambda e, i=i: e.mul(out=X[:, i, :], in_=X[:, i, :], mul=ALPHA), [Bx[i]], [Bx[i]])

            P.region_begin(("R", li))
            Bf = new_bufs(live_FA, ["ys0", "ys1", "hTr", "yb2"])
            live_FA = Bf
            Bys = Bf[0:2]; BhTr = Bf[2]
            ysb = [FA[:, 0:1024], FA[:, 1024:2048]]
            hTr = FA[:, 2048:2816].bitcast(BF16).rearrange("p (c t) -> p c t", c=4)
            nst = CAP // 128
            xstm = [ARv[:, 24576 + k * 3072:24576 + (k + 1) * 3072].rearrange("p (s f) -> p s f", s=nst) for k in range(2)]
            xsT = [ARv[:, 30720 + k * 3072:30720 + (k + 1) * 3072].rearrange("p (c t) -> p c t", c=8) for k in range(2)]
            hTrs = [FA[:, 2048 + k * 768:2816 + k * 768].bitcast(BF16).rearrange("p (c t) -> p c t", c=4) for k in range(2)]
            BhTrs = [Bf[2], Bf[3]]

            def wviews(e_):
                s_ = e_ % 2
                base = s_ * 12288
                return (ARv[:, base:base + 4096].rearrange("p (c f) -> p c f", c=8),
                        ARv[:, base + 4096:base + 8192].rearrange("p (c f) -> p c f", c=8),
                        ARv[:, base + 8192:base + 12288].rearrange("p (c f) -> p c f", c=4))

            def ex_load_x(e_):
                s_ = e_ % 2
                dma("sp", xstm[s_], xs_h.ap()[e_ * CAP:(e_ + 1) * CAP, :].rearrange("(s p) f -> p s f", p=128), [BXS], [Bw[6 + s_]], "rxs%d" % s_)

            def ex_A(e_):
                s_ = e_ % 2
                Bxm, BxT_ = Bw[6 + s_], Bw[8 + s_]
                for st_ in range(nst):
                    for c in range(8):
                        tr(ps_t[:, c * 128:(c + 1) * 128], xstm[s_][:, st_, c * 128:(c + 1) * 128], [Bxm], [Bps_t])
                    cp(xsT[s_][:, :, st_ * 128:(st_ + 1) * 128], ps_t[:].rearrange("p (c t) -> p c t", c=8), [Bps_t], [BxT_, Bps_t],
                       eng=("dve" if st_ % 2 == 0 else "act"))

            def ex_B(e_):
                s_ = e_ % 2
                Wg, Wu, Wd = wviews(e_)
                Bg, Bu = Bw[3 * s_], Bw[3 * s_ + 1]
                BxT_ = Bw[8 + s_]
                hT_, BhT_ = hTrs[s_], BhTrs[s_]
                for fc in range(4):
                    jg = nxt("s", 2)
                    ju = nxt("a", 2)
                    for c in range(8):
                        mm(ps_s[jg][:, 0:CAP], Wg[:, c, fc * 128:(fc + 1) * 128], xsT[s_][:, c, :], c == 0, c == 7, [BxT_, Bg], [Bps_s[jg]])
                    for c in range(8):
                        mm(ps_a[ju][:, 0:CAP], Wu[:, c, fc * 128:(fc + 1) * 128], xsT[s_][:, c, :], c == 0, c == 7, [BxT_, Bu], [Bps_a[ju]])
                    jp = nxt("pt", 3)
                    act(PT[:, jp, 0:CAP], ps_s[jg][:, 0:CAP], AF.Silu, [Bps_s[jg]], [BPT[jp], Bps_s[jg]])
                    tt(hT_[:, fc, :], ps_a[ju][:, 0:CAP], PT[:, jp, 0:CAP], ALU.mult, [Bps_a[ju], BPT[jp]], [BhT_, Bps_a[ju]])

            def ex_C(e_):
                s_ = e_ % 2
                Wg, Wu, Wd = wviews(e_)
                Bd = Bw[3 * s_ + 2]
                hT_, BhT_ = hTrs[s_], BhTrs[s_]
                for st_ in range(nst):
                    ky = nxt("ys", 2)
                    for half in range(2):
                        j = nxt("p", 2)
                        for fc in range(4):
                            mm(ps_p[j][:, :], hT_[:, fc, st_ * 128:(st_ + 1) * 128], Wd[:, fc, half * 512:(half + 1) * 512], fc == 0, fc == 3,
                               [BhT_, Bd], [Bps_p[j]])
                        cp(ysb[ky][:, half * 512:(half + 1) * 512], ps_p[j][:, :], [Bps_p[j]], [Bys[ky], Bps_p[j]],
                           eng=("act" if half == 0 else "dve"))
                    r0 = e_ * CAP + st_ * 128
                    dma("sp", ys_h.ap()[r0:r0 + 128, :], ysb[ky], [Bys[ky]], [BYS], "rys%d" % ky)

            ex_load_x(0)
            ex_A(0)
            for e_ in range(NE):
                if e_ + 1 < NE:
                    ex_load_x(e_ + 1)
                    if e_ + 1 >= 2:
                        load_expert_w(e_ + 1)
                ex_B(e_)
                if e_ + 1 < NE:
                    ex_A(e_ + 1)
                ex_C(e_)
            P.handoff([Bf[2], Bf[3]], [Bf[2], Bf[3]])
            Byb8 = new_bufs(Bw, ["yb8_%d" % q for q in range(8)])
            live_AR = Bw + Byb8
            yb8 = ARv[:, 0:16384].bitcast(F32).rearrange("p (k f) -> p k f", k=8)
            ybs = [(yb8[:, q, :], Byb8[q]) for q in range(8)]
            for i in range(NT):
                for k_ in range(2):
                    kb_ = nxt("yb", 8)
                    yb, Byb = ybs[kb_]
                    P.dma("pool", lambda e, i=i, k_=k_, yb=yb: e.indirect_dma_start(
                        out=yb, out_offset=None, in_=ys_h.ap(),
                        in_offset=bass.IndirectOffsetOnAxis(ap=SLI[:, k_, i:i + 1], axis=0)),
                        [BYS, Bsli], [Byb], "rg%d" % kb_)
                    stt(X[:, i, :], yb, SLF[:, 2 + k_, i:i + 1], X[:, i, :], ALU.mult, ALU.add, [Byb, Bslf, Bx[i]], [Bx[i]])
            P.region_end()

            P.region_begin(("D", li))
            names = []
            for s_ in range(2):
                names += ["wg%d" % s_, "wu%d" % s_, "wd%d" % s_]
            Bw = new_bufs(live_AR + live_AR_tail[0], names + ["hT"])
            live_AR = Bw
            live_AR_tail[0] = []
            BhT = Bw[6]
            hT = ARv[:, 24576:32768].rearrange("p (c t) -> p c t", c=4)
            for e_ in range(NE):
                s_ = e_ % 2
                base = s_ * 12288
                Wg = ARv[:, base:base + 4096].rearrange("p (c f) -> p c f", c=8)
                Wu = ARv[:, base + 4096:base + 8192].rearrange("p (c f) -> p c f", c=8)
                Wd = ARv[:, base + 8192:base + 12288].rearrange("p (c f) -> p c f", c=4)
                Bg, Bu, Bd = Bw[3 * s_], Bw[3 * s_ + 1], Bw[3 * s_ + 2]
                dma("pool", Wg, wg_h.ap()[li, e_].rearrange("(c p) f -> p c f", p=128), [], [Bg], "wg%d" % s_)
                dma("pool", Wu, wu_h.ap()[li, e_].rearrange("(c p) f -> p c f", p=128), [], [Bu], "wu%d" % s_)
                dma("pool", Wd, wd_h.ap()[li, e_].rearrange("(c p) f -> p c f", p=128), [], [Bd], "wd%d" % s_)
                for tb in range(4):
                    rdx = [BxT[4 * tb + q] for q in range(4)]
                    for fc in range(4):
                        jg = nxt("s", 2)
                        ju = nxt("a", 2)
                        for c in range(8):
                            mm(ps_s[jg][:, :], Wg[:, c, fc * 128:(fc + 1) * 128], XT[:, c, tb * 512:(tb + 1) * 512], c == 0, c == 7,
                               rdx + [Bg], [Bps_s[jg]])
                        for c in range(8):
                            mm(ps_a[ju][:, :], Wu[:, c, fc * 128:(fc + 1) * 128], XT[:, c, tb * 512:(tb + 1) * 512], c == 0, c == 7,
                               rdx + [Bu], [Bps_a[ju]])
                        jp = nxt("pt", 3)
                        act(PT[:, jp, :], ps_s[jg][:, :], AF.Silu, [Bps_s[jg]], [BPT[jp], Bps_s[jg]])
                        tt(hT[:, fc, tb * 512:(tb + 1) * 512], ps_a[ju][:, :], PT[:, jp, :], ALU.mult, [Bps_a[ju], BPT[jp]],
                           [BhT, Bps_a[ju]])
                for i in range(NT):
                    for half in range(2):
                        j = nxt("p", 2)
                        for fc in range(4):
                            mm(ps_p[j][:, :], hT[:, fc, i * 128:(i + 1) * 128], Wd[:, fc, half * 512:(half + 1) * 512], fc == 0, fc == 3,
                               [BhT, Bd], [Bps_p[j]])
                        stt(X[:, i, half * 512:(half + 1) * 512], ps_p[j][:, :], GATES[:, i * 16 + e_:i * 16 + e_ + 1],
                            X[:, i, half * 512:(half + 1) * 512], ALU.mult, ALU.add, [Bps_p[j], Bgates, Bx[i]], [Bx[i], Bps_p[j]])
            P.region_end()
            lng, lnb, sc, Bln, Bsc = load_ln(li, 1)

            def post2(i):
                if li == DEPTH - 1:
                    dma("sp", y_h.ap()[i * 128:(i + 1) * 128, :], X[:, i, :], [Bx[i]], [Bydram], "x%d" % i)
                else:
                    make_xT(i)

            ln_batched(lambda i: None, post2, lng, lnb, Bln)

        names = ["QT%d_%d" % (p, t) for p in range(8) for t in range(4)] + ["KT%d" % t for t in range(NT)] + \
                ["V%d" % t for t in range(NT)] + ["w0", "w1"]
        Bn = new_bufs(live_AR, names)
        live_AR = Bn[:64]
        live_AR_tail[0] = Bn[64:]
        BQT = [[Bn[p * 4 + t] for t in range(4)] for p in range(8)]
        BKT = Bn[32:48]; BV = Bn[48:64]; BW = Bn[64:66]
        QT = ARv[:, 0:16384].rearrange("p (c t) -> p c t", c=8)
        KT = ARv[:, 16384:20480].rearrange("p (c t) -> p c t", c=2)
        VA = ARv[:, 20480:28672].rearrange("p (t h d) -> p t h d", t=16, h=4)
        WS = [ARv[:, 28672 + s_ * 4096:28672 + (s_ + 1) * 4096].rearrange("p (c f) -> p c f", c=8) for s_ in range(2)]
        memset(ARv[:, 20480:28672], 1.0, BV, eng="pool")
        BFn = new_bufs(live_FA, ["G", "S1", "S2", "S3"])
        live_FA = BFn
        BG, BS1, BS2, BS3 = BFn
        G0 = FA[:, 0:512]; G1 = FA[:, 512:1024]; S1 = FA[:, 1024:1536]; S2 = FA[:, 1536:2048]; S3 = FA[:, 2048:2560]
        dma("sp", G0.rearrange("p (h d) -> p h d", h=8), bass.AP(aqn_h, 0, [[0, 128], [0, 8], [1, 64]]), [], [BG], "fa")
        dma("sp", G1[:, 0:256].rearrange("p (h d) -> p h d", h=4), bass.AP(aqn_h, 0, [[0, 128], [0, 4], [1, 64]]), [], [BG], "fa")
        dma("sp", G1[:, 256:512].rearrange("p (h d) -> p h d", h=4), bass.AP(akn_h, 0, [[0, 128], [0, 4], [1, 64]]), [], [BG], "fa")

        BSb = new_bufs(live_FA, ["S1b", "S2b", "S3b"])
        live_FA = live_FA + BSb
        SSETS = [(1024, 1536, 2048, BS1, BS2, BS3), (2560, 3072, 3584, BSb[0], BSb[1], BSb[2])]
        cnt["sset"] = 0

        def rms_rope1(j, i, G, kcs):
            k = nxt("st", 4)
            sset = SSETS[nxt("sset", 2)]
            o1, o2, o3, B1, B2, B3 = sset
            s1 = FA[:, o1:o1 + 512]
            ss = stt_[:, k, 0:8]; B_ = Bst[k]
            act(s1, ps_p[j][:, :], AF.Square, [Bps_p[j]], [B1, Bps_p[j]])
            red(ss, s1.rearrange("p (h d) -> p h d", h=8), ALU.add, [B1], [B_])
            ts(ss, ss, 1.0 / 64, EPS, ALU.mult, ALU.add, [B_], [B_])
            act(ss, ss, AF.Sqrt, [B_], [B_])
            recip(ss, ss, [B_], [B_])
            tt(s1.rearrange("p (h d) -> p h d", h=8), ps_p[j][:, :].rearrange("p (h d) -> p h d", h=8),
               ss.to_broadcast([128, 8, 64]), ALU.mult, [Bps_p[j], B_], [B1, Bps_p[j]])
            tt(s1, s1, G, ALU.mult, [B1, BG], [B1], eng="pool")
            return (sset, kcs)

        def rms_rope2(state):
            (o1, o2, o3, B1, B2, B3), kcs = state
            s1 = FA[:, o1:o1 + 512]; s2 = FA[:, o2:o2 + 512]; s3 = FA[:, o3:o3 + 512]
            tt(s2.rearrange("p (h d) -> p h d", h=8), s1.rearrange("p (h d) -> p h d", h=8),
               bass.AP(cs, kcs * 128, [[256, 128], [0, 8], [1, 64]]), ALU.mult, [B1, Bcs[kcs]], [B2])
            for hf in range(2):
                o = bass.AP(FA, o3 + hf * 16, [[4096, 128], [64, 8], [32, 2], [1, 16]])
                a = bass.AP(FA, o1 + (1 - hf) * 16, [[4096, 128], [64, 8], [32, 2], [1, 16]])
                b = bass.AP(cs, kcs * 128 + 64 + hf * 16, [[256, 128], [0, 8], [32, 2], [1, 16]])
                tt(o, a, b, ALU.mult, [B1, Bcs[kcs]], [B3], eng="pool")
            km = nxt("xq", 4)
            src = (xbf[:, km // 2, (km % 2) * 512:(km % 2) * 512 + 512], Bxq[km])
            tt(src[0], s2, s3, ALU.add, [B2, B3], [Bxq[km]])
            return src

        def tr_pairs(src, npair, src_off, dst_fn, wr_fn):
            sap, sbuf_ = src
            for pr in range(npair):
                tr(ps_t[:, pr * 128:(pr + 1) * 128], sap[:, src_off + pr * 128:src_off + (pr + 1) * 128], [sbuf_], [Bps_t])
            for pr in range(npair):
                cp(dst_fn(pr), ps_t[:, pr * 128:(pr + 1) * 128], [Bps_t], list(wr_fn(pr)) + [Bps_t], eng="act")

        def load_ws(cb):
            s_ = cb % 2
            dma("pool", WS[s_], awin_h.ap()[:, cb * 512:(cb + 1) * 512].rearrange("(c p) f -> p c f", p=128), [], [BW[s_]], "w%d" % s_)

        pend = []
        half2 = []

        def l0A(cb, i):
            s_ = cb % 2
            tsl = slice(i * 128, (i + 1) * 128)
            j = nxt("p", 2)
            for c in range(8):
                mm(ps_p[j][:, :], XT[:, c, tsl], WS[s_][:, c, :], c == 0, c == 7, [BxT[i], BW[s_]], [Bps_p[j]])
            if cb < 2:
                kcs = nxt("cs", 2)
                dma("sp", cs[:, kcs, 0:64], cosA_h.ap()[tsl, :], [], [Bcs[kcs]], "cs%d" % kcs)
                dma("sp", cs[:, kcs, 64:128], sinA_h.ap()[tsl, :], [], [Bcs[kcs]], "cs%d" % kcs)
                st_ = rms_rope1(j, i, G0 if cb == 0 else G1, kcs)

                def second(st_=st_, cb=cb, i=i, tsl=tsl):
                    kb = rms_rope2(st_)
                    if cb == 0:
                        pend.append(lambda: tr_pairs(kb, 4, 0, lambda pr: QT[:, pr, tsl], lambda pr: [BQT[pr][i // 4]]))
                    else:
                        def f():
                            tr_pairs(kb, 2, 0, lambda pr: QT[:, 4 + pr, tsl], lambda pr: [BQT[4 + pr][i // 4]])
                            tr_pairs(kb, 2, 256, lambda pr: KT[:, pr, tsl], lambda pr: [BKT[i]])
                        pend.append(f)
                half2.append(second)
            else:
                cp(VA[:, i, 0:2, 0:64], ps_p[j][:, 0:128].rearrange("p (h d) -> p h d", h=2), [Bps_p[j]], [BV[i], Bps_p[j]])
                cp(VA[:, i, 2:4, 64:128], ps_p[j][:, 128:256].rearrange("p (h d) -> p h d", h=2), [Bps_p[j]], [BV[i], Bps_p[j]])
                km = nxt("xq", 4)
                kb = (xbf[:, km // 2, (km % 2) * 512:(km % 2) * 512 + 512], Bxq[km])
                cp(kb[0][:, 0:256], ps_p[j][:, 256:512], [Bps_p[j]], [Bxq[km], Bps_p[j]], eng="act")
                pend.append(lambda: tr_pairs(kb, 2, 0, lambda pr: QT[:, 6 + pr, tsl], lambda pr: [BQT[6 + pr][i // 4]]))

        cnt["cs"] = 0
        cnt["xq"] = 0
        Bxq = new_bufs(Bxbf, ["xq%d" % q for q in range(4)])
        load_ws(0)
        load_ws(1)
        for cb in range(3):
            if cb == 1:
                pass
            for i in range(NT):
                if cb == 1 and i == 0:
                    load_ws(2)
                if len(pend) >= 2:
                    pend.pop(0)()
                prev2 = half2.pop(0) if half2 else None
                l0A(cb, i)
                if prev2 is not None:
                    prev2()
        while half2:
            half2.pop(0)()
        while pend:
            pend.pop(0)()
        P.handoff(Bxq + Bxbf, Bxbf)

        rec = FA[:, 2560:3072]
        Brec = new_bufs(BSb, ["rec"])[0]
        heads = []
        for p in range(8):
            for tb in range(4):
                for half in range(2):
                    rows = slice(0, 64) if half == 0 else slice(64, 128)
                    orow = slice(64, 128) if half == 0 else slice(0, 64)
                    qsl = slice(tb * 512, (tb + 1) * 512)
                    kp = p // 3 if p < 6 else p - 6
                    hv = kp if half == 0 else 2 + kp
                    if p < 6:
                        heads.append(dict(q_ap=QT[rows, p, qsl], k_fn=(lambda kt, rows=rows, kp=kp: KT[rows, kp, kt * 128:(kt + 1) * 128]),
                                          v_fn=(lambda kt, hv=hv: VA[:, kt, hv, :]), nkt=16, out_ap=XT[rows, p, qsl], rows=rows, orow=orow,
                                          rd_q=[BQT[p][tb]], rd_k=BKT, rd_v=BV, wr_out=[BxT[4 * tb + q] for q in range(4)]))
                    else:
                        heads.append(dict(q_ap=QT[rows, p, qsl], k_fn=(lambda kt, rows=rows, kp=kp: memKT[rows, kp, kt * 128:(kt + 1) * 128]),
                                          v_fn=(lambda kt, hv=hv: memV[:, kt, hv, :]), nkt=2, out_ap=XT[rows, p, qsl], rows=rows, orow=orow,
                                          rd_q=[BQT[p][tb]], rd_k=[BmemKT], rd_v=[BmemV], wr_out=[BxT[4 * tb + q] for q in range(4)]))
        ring4 = [(ps_s[0], Bps_s[0]), (ps_s[1], Bps_s[1]), (ps_p[0], Bps_p[0]), (ps_p[1], Bps_p[1])]
        ring2 = [(ps_s[0], Bps_s[0]), (ps_s[1], Bps_s[1])]
        memset(FA[:, 1024:2048], 0.0, [BS1, BS2])
        zero_fill()
        precast_weights(0)
        accs4 = [(ps_a[0][:, :], Bps_a[0]), (ps_a[1][:, :], Bps_a[1]), (ps_m[:, :], Bps_m), (ps_t[:].bitcast(F32), Bps_t)]
        run_attn(heads, rec, Brec, ring4, 2, accs4)
        live_FA = live_FA + [Brec]
        out_proj_ln(0, 8, awout_h, lambda p, i: XT[:, p, i * 128:(i + 1) * 128], lambda p, i: [BxT[i]], True)
        moe(0)

        names = ["OT%d_%d" % (p, t) for p in range(6) for t in range(NT)] + ["uw0", "uw1", "QTu0", "KTu0", "Vu0", "QTu1", "KTu1", "Vu1"]
        Bn = new_bufs(live_AR + live_AR_tail[0], names)
        live_AR = Bn
        live_AR_tail[0] = []
        BOT = [[Bn[p * NT + t] for t in range(NT)] for p in range(6)]
        BUW = Bn[96:98]
        BQTu = [Bn[98], Bn[101]]; BKTu = [Bn[99], Bn[102]]; BVu = [Bn[100], Bn[103]]
        OT = ARv[:, 0:12288].rearrange("p (c t) -> p c t", c=6)
        UW = [ARv[:, 12288 + s_ * 3072:12288 + (s_ + 1) * 3072].rearrange("p (c f) -> p c f", c=8) for s_ in range(2)]
        QTu = [ARv[:, 18432 + 8192 * k:20480 + 8192 * k] for k in range(2)]
        KTu = [ARv[:, 20480 + 8192 * k:22528 + 8192 * k] for k in range(2)]
        Vu = [ARv[:, 22528 + 8192 * k:26624 + 8192 * k].rearrange("p (t h d) -> p t h d", t=16, h=2) for k in range(2)]
        for k in range(2):
            memset(ARv[:, 22528 + 8192 * k:26624 + 8192 * k], 1.0, [BVu[k]], eng="pool")
        BFn = new_bufs(live_FA, ["accA", "accB"])
        live_FA = BFn
        BaccA, BaccB = BFn
        accA = FA[:, 0:2048]; accB = FA[:, 2048:4096]
        DIL = (1, 4, 16)
        units = [(pp, g) for pp in range(4) for g in range(3)]
        NU = len(units)

        def load_uw(n):
            pp, g = units[n]
            u = g * 4 + pp
            s_ = n % 2
            dma("pool", UW[s_], bwin_h.ap()[:, u * 384:(u + 1) * 384].rearrange("(c p) f -> p c f", p=128), [], [BUW[s_]], "uw%d" % s_)

        pj = {}

        def projA(n, jt):
            pp, g = units[n]
            d = DIL[g]
            tps = (S // d) // 128
            s_ = n % 2
            k = n % 2
            r = jt // tps
            l0 = (jt % tps) * 128
            kcs = nxt("tb", 2)
            dma("sp", tb16[:, kcs, :], tabB_h.ap()[g, jt * 128:(jt + 1) * 128, :], [], [Btb[kcs]], "tb%d" % kcs)
            j = nxt("p", 2)
            t0 = l0 * d + r
            rdx = [BxT[q] for q in range(t0 // 128, (t0 + 127 * d) // 128 + 1)]
            for c in range(8):
                mm(ps_p[j][:, 0:384], XT[:, c, t0:t0 + 127 * d + 1:d], UW[s_][:, c, :], c == 0, c == 7, rdx + [BUW[s_]], [Bps_p[j]])
            km = nxt("xm", 4)
            kb = km // 2
            xo = (km % 2) * 256
            Bxm_ = Bxmini[km]
            pj[(n, jt)] = km
            STG = RT[:, 4:6, :].rearrange("p a b -> p (a b)")
            cp(xbf[:, kb, xo:xo + 256], ps_p[j][:, 0:256], [Bps_p[j]], [Bxm_, Bps_p[j]], eng="act")
            cp(STG[:, 0:384], ps_p[j][:, 0:384], [Bps_p[j]], [Bstg, Bps_p[j]], eng="act")
            psv = STG[:, 0:256].rearrange("p (h d) -> p h d", h=4)
            T1 = bass.AP(tb16, kcs * 32, [[64, 128], [0, 4], [1, 16]])
            SA = RT[:, 0, 0:64].rearrange("p (h d) -> p h d", h=4)
            SB = RT[:, 1, 0:64].rearrange("p (h d) -> p h d", h=4)
            tt(SA, psv[:, :, 0:16], T1, ALU.mult, [Bstg, Btb[kcs]], [BRTa], eng="pool")
            tt(SB[:, :, 0:8], psv[:, :, 8:16],
               bass.AP(tb16, kcs * 32 + 16, [[64, 128], [0, 4], [1, 8]]), ALU.mult, [Bstg, Btb[kcs]], [BRTb], eng="pool")
            tt(SB[:, :, 8:16], psv[:, :, 0:8],
               bass.AP(tb16, kcs * 32 + 24, [[64, 128], [0, 4], [1, 8]]), ALU.mult, [Bstg, Btb[kcs]], [BRTb], eng="pool")
            tt(xbf[:, kb, xo:xo + 256].rearrange("p (h d) -> p h d", h=4)[:, :, 0:16], SA, SB, ALU.add, [BRTa, BRTb, Bxm_], [Bxm_], eng="pool")
            cp(Vu[k][:, jt, 0, 0:64], STG[:, 256:320], [Bstg], [BVu[k]], eng="pool")
            cp(Vu[k][:, jt, 1, 64:128], STG[:, 320:384], [Bstg], [BVu[k]], eng="pool")

        def projB(n, jt):
            k = n % 2
            km = pj.pop((n, jt))
            kb = km // 2
            xo = (km % 2) * 256
            tr(ps_t[:, 0:128], xbf[:, kb, xo:xo + 128], [Bxmini[km]], [Bps_t])
            tr(ps_t[:, 128:256], xbf[:, kb, xo + 128:xo + 256], [Bxmini[km]], [Bps_t])
            cp(QTu[k][:, jt * 128:(jt + 1) * 128], ps_t[:, 0:128], [Bps_t], [BQTu[k], Bps_t], eng="dve")
            cp(KTu[k][:, jt * 128:(jt + 1) * 128], ps_t[:, 128:256], [Bps_t], [BKTu[k], Bps_t], eng="dve")

        def proj_step(n, t):
            if t < NT:
                projA(n, t)
            if t >= 2:
                projB(n, t - 2)

        def kts_of(n, jt):
            pp, g = units[n]
            tps = (S // DIL[g]) // 128
            lo = (jt // tps) * tps
            hi = lo + tps - 1
            return [(jt + o, 1 + o) for o in (-1, 0, 1) if lo <= jt + o <= hi]

        qk = {}

        def attn_qk(n, jt, half):
            k = n % 2
            rows = slice(0, 64) if half == 0 else slice(64, 128)
            js = nxt("s3", 3)
            qk[(n, jt, half)] = js
            t_, b_ = ring3[js]
            for m, (kt, mi) in enumerate(kts_of(n, jt)):
                mm(t_[:, m * 128:(m + 1) * 128], KTu[k][rows, kt * 128:(kt + 1) * 128], QTu[k][rows, jt * 128:(jt + 1) * 128],
                   True, True, [BQTu[k], BKTu[k]], [b_])

        ja_cur = [0]

        ptk = {}
        PT4v = PT[:].rearrange("p a b -> p (a b)")

        def attn_pre(n, jt, half):
            kts = kts_of(n, jt)
            m0 = kts[0][1]
            nn = len(kts) * 128
            js = qk.pop((n, jt, half))
            jp = nxt("pt4", 4)
            ptk[(n, jt, half)] = jp
            pt = PT4v[:, jp * 384:jp * 384 + nn]
            t_, b_ = ring3[js]
            act(pt, t_[:, 0:nn], AF.Exp, [b_], [BPT4[jp], b_], scale=0.125)
            tt(pt, pt, maskb[:, m0 * 128:m0 * 128 + nn], ALU.mult, [BPT4[jp], Bmask], [BPT4[jp]], eng="dve")

        def attn_post(n, jt, half):
            pp, g = units[n]
            d = DIL[g]
            tps = (S // d) // 128
            k = n % 2
            kts = kts_of(n, jt)
            if half == 0:
                ja_cur[0] = nxt("a", 2)
            ja = ja_cur[0]
            jp = ptk.pop((n, jt, half))
            for m, (kt, mi) in enumerate(kts):
                mm(ps_a[ja][:, half * 128:(half + 1) * 128], Vu[k][:, kt, half, :], PT4v[:, jp * 384 + m * 128:jp * 384 + (m + 1) * 128],
                   m == 0, m == len(kts) - 1, [BPT4[jp], BVu[k]], [Bps_a[ja]])
            if half == 1:
                r = jt // tps
                l0 = (jt % tps) * 128
                t0 = l0 * d + r
                for hf, acc, Bacc in ((0, accA, BaccA), (1, accB, BaccB)):
                    dst = acc[:, t0:t0 + 127 * d + 1:d]
                    src = ps_a[ja][:, hf * 128:(hf + 1) * 128]
                    if g == 0:
                        cp(dst, src, [Bps_a[ja]], [Bacc, Bps_a[ja]])
                    else:
                        tt(dst, dst, src, ALU.add, [Bacc, Bps_a[ja]], [Bacc, Bps_a[ja]])

        def finish_pair(pp):
            recf = RT[:, 2:4, :].rearrange("p a b -> p (a b)")
            for blk in range(4):
                bs = slice(blk * 512, (blk + 1) * 512)
                for half, acc, Bacc in ((0, accA, BaccA), (1, accB, BaccB)):
                    rows = slice(0, 64) if half == 0 else slice(64, 128)
                    js = nxt("s", 2)
                    mm(ps_s[js][:, :], swpf[:], acc[:, bs], True, True, [Bacc, Bswp], [Bps_s[js]])
                    recip(recf[rows, :], ps_s[js][rows, :], [Bps_s[js]], [BRT, Bps_s[js]])
                    tt(OT[rows, pp, bs], acc[rows, bs], recf[rows, :], ALU.mult, [Bacc, BRT], BOT[pp][4 * blk:4 * blk + 4])

        cnt["tb"] = 0
        cnt["xm"] = 0
        cnt["s3"] = 0
        cnt["pt4"] = 0
        BPT4 = new_bufs(BPT, ["pt4_%d" % q for q in range(4)])
        ring3 = [(ps_s[0], Bps_s[0]), (ps_s[1], Bps_s[1]), (ps_m, Bps_m)]
        Bxmini = new_bufs(Bxbf, ["xm%d" % q for q in range(4)])
        BRTa = Buf("RTa"); BRTb = Buf("RTb"); Bstg = Buf("stg")
        P.handoff([BRT], [BRTa, BRTb, Bstg])
        load_uw(0)
        load_uw(1)
        for t in range(NT + 2):
            proj_step(0, t)
        for n in range(NU):
            pp, g = units[n]
            if n + 2 < NU:
                load_uw(n + 2)
            if n == NU - 1:
                dma("pool", UW[0][:, :, 0:256], bwin_h.ap()[:, 4608:4864].rearrange("(c p) f -> p c f", p=128), [], [BUW[0]], "uw0")
            precast_weights(1, 4 * n, 4 * n + 4)
            asteps = [(jt, half) for jt in range(NT) for half in range(2)]
            attn_qk(n, 0, 0)
            attn_qk(n, 0, 1)
            attn_pre(n, 0, 0)
            attn_pre(n, 0, 1)
            for t in range(NT + 2):
                if t < NT:
                    for half in range(2):
                        si = t * 2 + half
                        if si + 2 < len(asteps):
                            attn_qk(n, *asteps[si + 2])
                        attn_post(n, t, half)
                    if t + 1 < NT:
                        attn_pre(n, t + 1, 0)
                        attn_pre(n, t + 1, 1)
                if n + 1 < NU:
                    proj_step(n + 1, t)
            if g == 2:
                finish_pair(pp)
        P.handoff([BRTa, BRTb, Bstg, BRT], [BRT])
        P.handoff(Bxmini + Bxbf, Bxbf)
        P.handoff(BPT4 + BPT, BPT)
        ucount = NU
        BQTu0, BKTu0 = BQTu[0], BKTu[0]
        s_ = 0
        QTm = ARv[:, 18432:22528].rearrange("p (c t) -> p c t", c=2)
        Bqm = new_bufs([BQTu0, BKTu0], ["QTm%d" % t for t in range(4)])
        for i in range(NT):
            tsl = slice(i * 128, (i + 1) * 128)
            j = nxt("p", 2)
            for c in range(8):
                mm(ps_p[j][:, 0:256], XT[:, c, tsl], UW[s_][:, c, 0:256], c == 0, c == 7, [BxT[i], BUW[s_]], [Bps_p[j]])
            kb = nxt("xbf", 2)
            cp(xbf[:, kb, 0:256], ps_p[j][:, 0:256], [Bps_p[j]], [Bxbf[kb], Bps_p[j]], eng="act")
            tr_pairs((xbf[:, kb, :], Bxbf[kb]), 2, 0, lambda pr: QTm[:, pr, tsl], lambda pr: [Bqm[i // 4]])
        ring4b = [(ps_s[0], Bps_s[0]), (ps_s[1], Bps_s[1]), (ps_p[0], Bps_p[0]), (ps_p[1], Bps_p[1])]
        rec = S3
        heads = []
        for kp in range(2):
            for tb in range(4):
                for half in range(2):
                    rows = slice(0, 64) if half == 0 else slice(64, 128)
                    orow = slice(64, 128) if half == 0 else slice(0, 64)
                    qsl = slice(tb * 512, (tb + 1) * 512)
                    hv = kp if half == 0 else 2 + kp
                    heads.append(dict(q_ap=QTm[rows, kp, qsl], k_fn=(lambda kt, rows=rows, kp=kp: memKT[rows, kp, kt * 128:(kt + 1) * 128]),
                                      v_fn=(lambda kt, hv=hv: memV[:, kt, hv, :]), nkt=2, out_ap=OT[rows, 4 + kp, qsl], rows=rows, orow=orow,
                                      rd_q=[Bqm[tb]], rd_k=[BmemKT], rd_v=[BmemV], wr_out=[BOT[4 + kp][4 * tb + q] for q in range(4)]))
        run_attn(heads, rec, BaccB, ring4b, 2)
        live_AR = live_AR + Bqm
        out_proj_ln(1, 6, bwout_h, lambda p, i: OT[:, p, i * 128:(i + 1) * 128], lambda p, i: [BOT[p][i]], False)
        moe(1)
        P.finish()
        P.emit(nc, st, regs)
    return nc


def _tables():
    half = 32
    inv = 1.0 / (10000.0 ** (np.arange(0, half, 2, dtype=np.float32) / half))
    t = np.arange(S)
    row = (t // 64).astype(np.float32)[:, None] * inv[None, :]
    col = (t % 64).astype(np.float32)[:, None] * inv[None, :]
    cr, sr, cc, sc = np.cos(row), np.sin(row), np.cos(col), np.sin(col)
    cosA = np.concatenate([cr, cr, cc, cc], 1).astype(np.float32)
    sinA = np.concatenate([-sr, sr, -sc, sc], 1).astype(np.float32)
    invb = 1.0 / (500000.0 ** (np.arange(0, 16, 2, dtype=np.float32) / 16))
    tabB = np.zeros((3, S, 32), np.float32)
    for g, d in enumerate((1, 4, 16)):
        L = S // d
        i = np.arange(S)
        tok = (i % L) * d + (i // L)
        ang = tok.astype(np.float32)[:, None] * invb[None, :]
        c, s = np.cos(ang), np.sin(ang)
        tabB[g] = np.concatenate([c, c, -s, s], 1)
    k = np.arange(128)[:, None]
    q = np.arange(128)[None, :]
    m_prev = (k >= q + 64)
    m_own = (np.abs(q - k) <= 64)
    m_next = (k <= q - 64)
    masks = np.concatenate([m_prev, m_own, m_next], 1).astype(np.float32)
    return cosA, sinA, tabB, masks


_NC_CACHE = {}


def kernel(x, mem, w_mem_kv, router_w, router_b, a_w_in, a_w_out, a_q_norm, a_k_norm,
           b_w_in, b_w_out, ln1_g, ln1_b, ln2_g, ln2_b, w_gate, w_up, w_down):
    f = lambda a: np.ascontiguousarray(np.asarray(a, dtype=np.float32))
    x, mem = f(x), f(mem)
    hd = 64
    wkv = f(w_mem_kv)
    kperm = [0, 2, 1, 3]
    wkv_p = np.concatenate([wkv[:, h * hd:(h + 1) * hd] for h in kperm] + [wkv[:, 256:512]], 1)
    awin = f(a_w_in)[0]
    qc = lambda h: awin[:, h * hd:(h + 1) * hd]
    kc = lambda h: awin[:, 768 + h * hd:768 + (h + 1) * hd]
    vc = lambda h: awin[:, 1024 + h * hd:1024 + (h + 1) * hd]
    mc = lambda h: awin[:, 1280 + h * hd:1280 + (h + 1) * hd]
    cols = [qc(0), qc(6), qc(1), qc(7), qc(2), qc(8), qc(3), qc(9),
            qc(4), qc(10), qc(5), qc(11), kc(0), kc(2), kc(1), kc(3),
            vc(0), vc(1), vc(2), vc(3), mc(0), mc(2), mc(1), mc(3)]
    awin_p = np.ascontiguousarray(np.concatenate(cols, 1))
    awout = f(a_w_out)[0]
    rows = []
    for p in range(6):
        rows += [awout[p * hd:(p + 1) * hd], awout[(6 + p) * hd:(7 + p) * hd]]
    for a, b in ((0, 2), (1, 3)):
        rows += [awout[768 + a * hd:768 + (a + 1) * hd], awout[768 + b * hd:768 + (b + 1) * hd]]
    awout_p = np.ascontiguousarray(np.concatenate(rows, 0))
    bwin = f(b_w_in)[0]
    cols = []
    for g in range(3):
        for pp in range(4):
            for part in range(3):
                for h in (2 * pp, 2 * pp + 1):
                    o = part * 1536 + g * 512 + h * hd
                    cols.append(bwin[:, o:o + hd])
    for h in kperm:
        cols.append(bwin[:, 4608 + h * hd:4608 + (h + 1) * hd])
    bwin_p = np.ascontiguousarray(np.concatenate(cols, 1))
    bwout = f(b_w_out)[0]
    rows = [bwout[0:512]]
    for a, b in ((0, 2), (1, 3)):
        rows += [bwout[512 + a * hd:512 + (a + 1) * hd], bwout[512 + b * hd:512 + (b + 1) * hd]]
    bwout_p = np.ascontiguousarray(np.concatenate(rows, 0))
    lnv = np.stack([f(ln1_g)[0], f(ln1_b)[0], f(ln2_g)[0], f(ln2_b)[0],
                    f(ln1_g)[1], f(ln1_b)[1], f(ln2_g)[1], f(ln2_b)[1]], 0)
    cosA, sinA, tabB, masks = _tables()
    shared = {"wkv": wkv_p, "rw": f(router_w), "rb": f(router_b).reshape(1, 16), "awin": awin_p, "awout": awout_p,
              "aqn": f(a_q_norm).reshape(1, 64), "akn": f(a_k_norm).reshape(1, 64), "bwin": bwin_p, "bwout": bwout_p,
              "lnv": np.ascontiguousarray(lnv), "wg": f(w_gate), "wu": f(w_up), "wd": f(w_down),
              "cosA": cosA, "sinA": sinA, "tabB": tabB, "masks": masks,
              "swp": np.ascontiguousarray(np.roll(np.eye(128, dtype=np.float32), 64, axis=1)),
              "ltri": np.ascontiguousarray(np.triu(np.ones((128, 128), np.float32), 1)),
              "ecap": (np.arange(16, dtype=np.float32) * CAP).reshape(1, 16)}
    if "nc" not in _NC_CACHE:
        _NC_CACHE["nc"] = build()
    nc = _NC_CACHE["nc"]
    in_maps = []
    for b in range(8):
        m = dict(shared)
        m["x"] = x[b]
        m["mem"] = mem[b]
        in_maps.append(m)
    res = run_bass_kernel_spmd(nc, in_maps, core_ids=list(range(8)))
    return np.stack([np.asarray(r["y"], dtype=np.float32) for r in res.results], 0)
```

```python
import bisect
import math
from contextlib import ExitStack
import numpy as np
import concourse.bass as bass
import concourse.mybir as mybir
from concourse.bass_utils import run_bass_kernel_spmd

F32 = mybir.dt.float32
BF16 = mybir.dt.bfloat16
AF = mybir.ActivationFunctionType
ALU = mybir.AluOpType
AX = mybir.AxisListType

S = 2048
D = 1024
NT = 16
NE = 16
DEPTH = 2
ALPHA = (2 * DEPTH) ** 0.25
EPS = 1e-6
CAP = 384
I32 = mybir.dt.int32
ENGS = ("pe", "act", "dve", "pool", "sp")


class Buf:
    __slots__ = ("name", "w", "r")

    def __init__(self, name):
        self.name = name
        self.w = None
        self.r = []


class Ins:
    __slots__ = ("eng", "fn", "waits", "inc", "pos", "incval", "dma_owner", "region")

    def __init__(self, eng, fn):
        self.eng = eng
        self.fn = fn
        self.region = None
        self.waits = []
        self.inc = False
        self.incval = None
        self.dma_owner = None


class Prog:
    def __init__(self):
        self.streams = {e: [] for e in ENGS}
        self.inc_pos = {e: [] for e in ENGS}
        self.inc_cnt = {e: 0 for e in ENGS}
        self.waited = {e: {} for e in ENGS}
        self.dma_owners = {}
        self.region = None

    def region_begin(self, r):
        self.region = r
        self.waited = {e: {} for e in ENGS}

    def region_end(self):
        self.region = None
        self.waited = {e: {} for e in ENGS}

    def _resolve(self, tok, eng):
        if tok[0] == "c":
            ins = tok[1]
            if ins.eng == eng and eng == "pe":
                return None
            if ins.incval is None:
                lst = self.inc_pos[ins.eng]
                i = bisect.bisect_left(lst, ins.pos)
                if i < len(lst):
                    return (ins.eng, self.streams[ins.eng][lst[i]].incval)
                self.inc_cnt[ins.eng] += 1
                ins.incval = self.inc_cnt[ins.eng]
                ins.inc = True
                lst.append(ins.pos)
            return (ins.eng, ins.incval)
        return (("dma", tok[1]), tok[2])

    def _deps(self, ins, reads, writes):
        latest = {}
        need = {}

        def add(tok):
            if tok is None:
                return
            if tok[0] == "c":
                p = tok[1]
                if p.eng == ins.eng and ins.eng == "pe":
                    return
                cur = latest.get(p.eng)
                if cur is None or cur.pos < p.pos:
                    latest[p.eng] = p
            else:
                key = ("dma", tok[1])
                if need.get(key, 0) < tok[2]:
                    need[key] = tok[2]

        for b in reads:
            add(b.w)
        for b in writes:
            add(b.w)
            for t in b.r:
                add(t)
        for p in latest.values():
            r = self._resolve(("c", p), ins.eng)
            if r is not None:
                need[r[0]] = max(need.get(r[0], 0), r[1])
        w = self.waited[ins.eng]
        for key, val in need.items():
            if w.get(key, 0) >= val:
                continue
            w[key] = val
            ins.waits.append((key, val))

    def _commit(self, tok, reads, writes):
        for b in reads:
            b.r.append(tok)
        for b in writes:
            b.w = tok
            b.r = []

    def op(self, eng, fn, reads=(), writes=()):
        ins = Ins(eng, fn)
        ins.region = self.region
        ins.pos = len(self.streams[eng])
        self._deps(ins, reads, writes)
        self.streams[eng].append(ins)
        self._commit(("c", ins), reads, writes)
        return ins

    def dma(self, eng, fn, reads=(), writes=(), owner=None):
        ins = Ins(eng, fn)
        ins.region = self.region
        ins.pos = len(self.streams[eng])
        self._deps(ins, reads, writes)
        okey = owner + "@" + eng
        cnt = self.dma_owners.get(okey, 0) + 1
        self.dma_owners[okey] = cnt
        ins.dma_owner = okey
        self.streams[eng].append(ins)
        self._commit(("d", okey, 16 * cnt), reads, writes)
        return ins

    def handoff(self, olds, news):
        toks = []
        for b in olds:
            if b.w is not None:
                toks.append(b.w)
            toks.extend(b.r)
        for b in news:
            b.w = None
            b.r = list(toks)

    def finish(self):
        ins = Ins("sp", None)
        ins.pos = len(self.streams["sp"])
        for name, cnt in self.dma_owners.items():
            key = ("dma", name)
            if self.waited["sp"].get(key, 0) < 16 * cnt:
                self.waited["sp"][key] = 16 * cnt
                ins.waits.append((key, 16 * cnt))
        self.streams["sp"].append(ins)

    def emit(self, nc, stack, regs):
        sems = {}
        for e in ("pe", "act", "dve", "pool"):
            sems[e] = stack.enter_context(nc.semaphore("sem_" + e))
        for name in self.dma_owners:
            sems[("dma", name)] = stack.enter_context(nc.semaphore("sd_" + name.replace("@", "_")))
        block = stack.enter_context(nc.Block())
        engmap = {"pe": block.tensor, "act": block.scalar, "dve": block.vector,
                  "pool": block.gpsimd, "sp": block.sync}
        for e in ENGS:
            stream = self.streams[e]

            def body(engine, stream=stream, e=e):
                done = {}

                def emit_ins(ins):
                    for key, val in ins.waits:
                        engine.wait_ge(sems[key], val)
                    if ins.fn is None:
                        return
                    bi = ins.fn(engine)
                    if ins.dma_owner is not None:
                        bi.then_inc(sems[("dma", ins.dma_owner)], 16)
                    elif ins.inc:
                        bi.then_inc(sems[e], 1)

                def account(ins, d):
                    if ins.dma_owner is not None:
                        k = ("dma", ins.dma_owner)
                        d[k] = d.get(k, 0) + 16
                    elif ins.inc:
                        d[e] = d.get(e, 0) + 1

                i = 0
                n = len(stream)
                while i < n:
                    r = stream[i].region
                    j = i
                    while j < n and stream[j].region == r:
                        j += 1
                    group = stream[i:j]
                    if r is None:
                        for ins in group:
                            emit_ins(ins)
                            account(ins, done)
                    else:
                        comp = {}
                        for ins in group:
                            account(ins, comp)
                        guard = engine.If_eq(regs[e], 0) if r[0] == "R" else engine.If_ne(regs[e], 0)
                        with guard:
                            for ins in group:
                                emit_ins(ins)
                        with engine.Else():
                            for k, amt in comp.items():
                                if done.get(k, 0) > 0:
                                    engine.wait_ge(sems[k], done[k])
                                engine.sem_inc(sems[k], amt)
                        for k, amt in comp.items():
                            done[k] = done.get(k, 0) + amt
                    i = j

            engmap[e](body)


def build(debug_stop=None):
    nc = bass.Bass("TRN2", target_bir_lowering=False)

    def din(name, shape):
        return nc.dram_tensor(name, list(shape), F32, kind="ExternalInput")

    x_h = din("x", [S, D]); mem_h = din("mem", [256, D]); wkv_h = din("wkv", [D, 512])
    rw_h = din("rw", [D, 16]); rb_h = din("rb", [1, 16])
    awin_h = din("awin", [D, 1536]); awout_h = din("awout", [1024, D])
    aqn_h = din("aqn", [1, 64]); akn_h = din("akn", [1, 64])
    bwin_h = din("bwin", [D, 4864]); bwout_h = din("bwout", [768, D])
    lnv_h = din("lnv", [8, D])
    wg_h = din("wg", [2, NE, D, 512]); wu_h = din("wu", [2, NE, D, 512]); wd_h = din("wd", [2, NE, 512, D])
    cosA_h = din("cosA", [S, 64]); sinA_h = din("sinA", [S, 64]); tabB_h = din("tabB", [3, S, 32])
    mask_h = din("masks", [128, 384])
    swp_h = din("swp", [128, 128])
    ltri_h = din("ltri", [128, 128]); ecap_h = din("ecap", [1, 16])
    NSLOT = NE * CAP
    xs_h = nc.dram_tensor("xs_scr", [NSLOT, D], BF16)
    ys_h = nc.dram_tensor("ys_scr", [NSLOT, D], F32)
    wbg_h = nc.dram_tensor("wbg_scr", [2, NE, D, 512], BF16)
    wbu_h = nc.dram_tensor("wbu_scr", [2, NE, D, 512], BF16)
    wbd_h = nc.dram_tensor("wbd_scr", [2, NE, 512, D], BF16)
    y_h = nc.dram_tensor("y", [S, D], F32, kind="ExternalOutput")

    P = Prog()
    st = ExitStack()
    with st:
        X = st.enter_context(nc.sbuf_tensor("X", [128, NT, D], F32))
        XT = st.enter_context(nc.sbuf_tensor("XT", [128, 8, S], BF16))
        AR = st.enter_context(nc.sbuf_tensor("AR", [128, 36864], BF16))
        FA = st.enter_context(nc.sbuf_tensor("FA", [128, 4096], F32))
        ident = st.enter_context(nc.sbuf_tensor("ident", [128, 128], BF16))
        xbf = st.enter_context(nc.sbuf_tensor("xbf", [128, 2, D], BF16))
        PT = st.enter_context(nc.sbuf_tensor("PT", [128, 3, 512], BF16))
        memKT = st.enter_context(nc.sbuf_tensor("memKT", [128, 2, 256], BF16))
        memV = st.enter_context(nc.sbuf_tensor("memV", [128, 2, 4, 128], BF16))
        rwb = st.enter_context(nc.sbuf_tensor("rwb", [128, 8, 16], BF16))
        rbt = st.enter_context(nc.sbuf_tensor("rbt", [128, 16], F32))
        maskb = st.enter_context(nc.sbuf_tensor("maskb", [128, 384], BF16))
        swpf = st.enter_context(nc.sbuf_tensor("swpf", [128, 128], F32))
        SLF = st.enter_context(nc.sbuf_tensor("SLF", [128, 4, 16], F32))
        SLI = st.enter_context(nc.sbuf_tensor("SLI", [128, 2, 16], I32))
        fi = st.enter_context(nc.sbuf_tensor("fi", [128, 2], I32))
        maskbf = st.enter_context(nc.sbuf_tensor("maskbf", [128, 256], BF16))
        ltri = st.enter_context(nc.sbuf_tensor("ltri_sb", [128, 128], BF16))
        onesm = st.enter_context(nc.sbuf_tensor("onesm", [128, 128], BF16))
        ecap = st.enter_context(nc.sbuf_tensor("ecap_sb", [128, 16], F32))
        regs = {"pe": st.enter_context(nc.tensor.register("flag_pe")), "act": st.enter_context(nc.scalar.register("flag_act")),
                "dve": st.enter_context(nc.vector.register("flag_dve")), "pool": st.enter_context(nc.gpsimd.register("flag_pool")),
                "sp": st.enter_context(nc.sync.register("flag_sp"))}
        stt_ = st.enter_context(nc.sbuf_tensor("stt", [128, 4, 16], F32))
        GATES = st.enter_context(nc.sbuf_tensor("GATES", [128, 256], F32))
        RT = st.enter_context(nc.sbuf_tensor("RT", [128, 6, 256], F32))
        tb16 = st.enter_context(nc.sbuf_tensor("tb16", [128, 2, 32], F32))
        cs = st.enter_context(nc.sbuf_tensor("cs", [128, 2, 128], F32))
        ps_s = [st.enter_context(nc.psum_tensor("ps_s%d" % i, [128, 512], F32)) for i in range(2)]
        ps_a = [st.enter_context(nc.psum_tensor("ps_a%d" % i, [128, 512], F32)) for i in range(2)]
        ps_p = [st.enter_context(nc.psum_tensor("ps_p%d" % i, [128, 512], F32)) for i in range(2)]
        ps_t = st.enter_context(nc.psum_tensor("ps_t", [128, 1024], BF16))
        ps_m = st.enter_context(nc.psum_tensor("ps_m", [128, 512], F32))

        Bx = [Buf("x%d" % i) for i in range(NT)]
        BxT = [Buf("xT%d" % i) for i in range(NT)]
        Bxbf = [Buf("xbf%d" % i) for i in range(2)]
        BPT = [Buf("PT%d" % i) for i in range(3)]
        Bps_s = [Buf("ps_s%d" % i) for i in range(2)]
        Bps_a = [Buf("ps_a%d" % i) for i in range(2)]
        Bps_p = [Buf("ps_p%d" % i) for i in range(2)]
        Bps_t = Buf("ps_t"); Bps_m = Buf("ps_m")
        Bident = Buf("ident"); BmemKT = Buf("memKT"); BmemV = Buf("memV"); Brw = Buf("rw"); Brb = Buf("rb")
        Bmask = Buf("mask"); Bst = [Buf("st%d" % i) for i in range(4)]; Bgates = Buf("gates"); BRT = Buf("RT")
        Btb = [Buf("tb%d" % i) for i in range(2)]; Bcs = [Buf("cs%d" % i) for i in range(2)]
        Bydram = Buf("ydram")
        live_AR = []
        live_FA = []

        def new_bufs(live, names):
            nb = [Buf(n) for n in names]
            P.handoff(live, nb)
            return nb

        def mm(out, lhsT, rhs, start, stop, rd, wr):
            P.op("pe", lambda e: e.matmul(out, lhsT=lhsT, rhs=rhs, start=start, stop=stop), rd, wr)

        def tr(out, in_, rd, wr):
            P.op("pe", lambda e: e.transpose(out=out, in_=in_, identity=ident[:]), list(rd) + [Bident], wr)

        def act(out, in_, func, rd, wr, scale=None, bias=None):
            kw = {}
            if scale is not None:
                kw["scale"] = scale
            if bias is not None:
                kw["bias"] = bias
            P.op("act", lambda e: e.activation(out=out, in_=in_, func=func, **kw), rd, wr)

        def tt(out, in0, in1, op, rd, wr, eng="dve"):
            P.op(eng, lambda e: e.tensor_tensor(out=out, in0=in0, in1=in1, op=op), rd, wr)

        def ts(out, in0, s1, s2, op0, op1, rd, wr, eng="dve"):
            if op1 is None:
                P.op(eng, lambda e: e.tensor_scalar(out=out, in0=in0, scalar1=s1, scalar2=None, op0=op0), rd, wr)
            else:
                P.op(eng, lambda e: e.tensor_scalar(out=out, in0=in0, scalar1=s1, scalar2=s2, op0=op0, op1=op1), rd, wr)

        def stt(out, in0, scalar, in1, op0, op1, rd, wr, eng="dve"):
            P.op(eng, lambda e: e.scalar_tensor_tensor(out=out, in0=in0, scalar=scalar, in1=in1, op0=op0, op1=op1), rd, wr)

        def cp(out, in_, rd, wr, eng="dve"):
            if eng == "act":
                P.op("act", lambda e: e.copy(out=out, in_=in_), rd, wr)
            else:
                P.op(eng, lambda e: e.tensor_copy(out=out, in_=in_), rd, wr)

        def red(out, in_, op, rd, wr):
            P.op("dve", lambda e: e.tensor_reduce(out=out, in_=in_, axis=AX.X, op=op), rd, wr)

        def recip(out, in_, rd, wr):
            P.op("dve", lambda e: e.reciprocal(out=out, in_=in_), rd, wr)

        def memset(out, val, wr, eng="dve"):
            P.op(eng, lambda e: e.memset(out, val), (), wr)

        def dma(eng, out, in_, rd, wr, owner):
            P.dma(eng, lambda e: e.dma_start(out=out, in_=in_), rd, wr, owner)

        cnt = {"s": 0, "a": 0, "p": 0, "pt": 0, "xbf": 0, "st": 0, "ys": 0, "yb": 0}

        def nxt(k, n):
            v = cnt[k] % n
            cnt[k] += 1
            return v

        memset(ident[:], 0.0, [Bident], eng="pool")
        P.op("pool", lambda e: e.affine_select(out=ident[:], in_=ident[:], pattern=[[-1, 128]], compare_op=ALU.not_equal,
                                               fill=1.0, base=0, channel_multiplier=1), [Bident], [Bident])
        memset(memV[:], 1.0, [BmemV], eng="pool")
        dma("pool", rwb[:], rw_h.ap().rearrange("(c p) f -> p c f", p=128), [], [Brw], "rw")
        dma("sp", rbt[:], bass.AP(rb_h, 0, [[0, 128], [1, 16]]), [], [Brb], "rb")
        Bswp = Buf("swp")
        dma("sp", swpf[:], swp_h.ap(), [], [Bswp], "swp")
        Bltri = Buf("ltri"); Bones = Buf("onesm"); Becap = Buf("ecap"); Bzt = Buf("zt"); BXS = Buf("XS"); BYS = Buf("YS")
        Bslf = Buf("SLF"); Bsli = Buf("SLI"); Bfi = [Buf("fi0"), Buf("fi1")]; Bmaskbf = Buf("maskbf")
        memset(onesm[:], 1.0, [Bones])
        dma("sp", ecap[:], bass.AP(ecap_h, 0, [[0, 128], [1, 16]]), [], [Becap], "ecap")
        BWB = [Buf("wb0"), Buf("wb1")]

        def precast_weights(li, lo=0, hi=3 * NE):
            jobs = [(e_, src, dst) for e_ in range(NE) for src, dst in ((wg_h, wbg_h), (wu_h, wbu_h), (wd_h, wbd_h))]
            for e_, src, dst in jobs[lo:hi]:
                dma("pool", dst.ap()[li, e_], src.ap()[li, e_], [], [BWB[li]], "wb%d" % li)

        def zero_fill():
            for hh, BB, nm in ((xs_h, BXS, "zx"), (ys_h, BYS, "zy")):
                v = hh.ap().rearrange("(j p) f -> p j f", p=128)
                nj = NSLOT // 128
                for j0 in range(0, nj, 6):
                    if hh is xs_h:
                        zs = FA[:, 1024:1536].bitcast(BF16)
                        src = bass.AP(zs.tensor, zs.offset, [[zs.ap[0][0], 128], [0, 6], [1, 1024]])
                    else:
                        src = bass.AP(FA, 1024, [[4096, 128], [0, 6], [1, 1024]])
                    dma("sp", v[:, j0:j0 + 6, :], src, [BS1, BS2], [BB], nm)

        BFA0 = new_bufs(live_FA, ["fa_init"])
        live_FA = BFA0
        dma("sp", FA[:, 0:384], mask_h.ap(), [], BFA0, "fa")
        cp(maskb[:], FA[:, 0:384], BFA0, [Bmask])
        dma("sp", FA[:, 512:640], ltri_h.ap(), [], BFA0, "fa")
        cp(ltri[:], FA[:, 512:640], BFA0, [Bltri])

        def make_xT(i, cast_eng="act"):
            k = nxt("xbf", 2)
            cp(xbf[:, k, :], X[:, i, :], [Bx[i]], [Bxbf[k]], eng=cast_eng)
            for c in range(8):
                tr(ps_t[:, c * 128:(c + 1) * 128], xbf[:, k, c * 128:(c + 1) * 128], [Bxbf[k]], [Bps_t])
            cp(XT[:, :, i * 128:(i + 1) * 128], ps_t[:].rearrange("p (c t) -> p c t", c=8), [Bps_t], [BxT[i], Bps_t])

        for i in range(NT):
            dma("sp", X[:, i, :], x_h.ap()[i * 128:(i + 1) * 128, :], [], [Bx[i]], "x%d" % i)
        for i in range(NT):
            make_xT(i)

        ARv = AR[:]
        BA = new_bufs(live_AR, ["memT", "wkv"])
        live_AR = BA
        memT = ARv[:, 0:2048].rearrange("p (c t) -> p c t", c=8)
        wkv = ARv[:, 2048:6144].rearrange("p (c f) -> p c f", c=8)
        dma("pool", wkv, wkv_h.ap().rearrange("(c p) f -> p c f", p=128), [], [BA[1]], "wkv")
        BF_ = new_bufs(live_FA, ["fa_mem"])
        live_FA = BF_
        for mt in range(2):
            dma("sp", FA[:, 0:1024], mem_h.ap()[mt * 128:(mt + 1) * 128, :], [], BF_, "fa")
            k = nxt("xbf", 2)
            cp(xbf[:, k, :], FA[:, 0:1024], BF_, [Bxbf[k]], eng="act")
            for c in range(8):
                tr(ps_t[:, c * 128:(c + 1) * 128], xbf[:, k, c * 128:(c + 1) * 128], [Bxbf[k]], [Bps_t])
            cp(memT[:, :, mt * 128:(mt + 1) * 128], ps_t[:].rearrange("p (c t) -> p c t", c=8), [Bps_t], [BA[0], Bps_t])
        for mt in range(2):
            j = nxt("p", 2)
            for c in range(8):
                mm(ps_p[j][:, :], memT[:, c, mt * 128:(mt + 1) * 128], wkv[:, c, :], c == 0, c == 7, [BA[0], BA[1]], [Bps_p[j]])
            k = nxt("xbf", 2)
            cp(xbf[:, k, 0:256], ps_p[j][:, 0:256], [Bps_p[j]], [Bxbf[k], Bps_p[j]], eng="act")
            for pr in range(2):
                tr(ps_t[:, pr * 128:(pr + 1) * 128], xbf[:, k, pr * 128:(pr + 1) * 128], [Bxbf[k]], [Bps_t])
            cp(memKT[:, :, mt * 128:(mt + 1) * 128], ps_t[:, 0:256].rearrange("p (c t) -> p c t", c=2), [Bps_t], [BmemKT, Bps_t])
            cp(memV[:, mt, 0:2, 0:64], ps_p[j][:, 256:384].rearrange("p (h d) -> p h d", h=2), [Bps_p[j]], [BmemV, Bps_p[j]])
            cp(memV[:, mt, 2:4, 64:128], ps_p[j][:, 384:512].rearrange("p (h d) -> p h d", h=2), [Bps_p[j]], [BmemV, Bps_p[j]])

        def run_attn(heads, rec_ap, Brec, ring, look, accs=None):
            if accs is None:
                accs = [(ps_a[0][:, :], Bps_a[0]), (ps_a[1][:, :], Bps_a[1])]
            steps = []
            for h0 in range(0, len(heads), 2):
                for kt in range(heads[h0]["nkt"]):
                    steps.append((h0, kt))
                    steps.append((h0 + 1, kt))
            sbuf_of = {}
            rc = [0]

            def issue_qk(idx):
                hi, kt = steps[idx]
                h = heads[hi]
                t_, b_ = ring[rc[0] % len(ring)]
                rc[0] += 1
                sbuf_of[idx] = (t_, b_)
                mm(t_[:, :], h["k_fn"](kt), h["q_ap"], True, True, list(h["rd_q"]) + list(h["rd_k"]), [b_])

            issue_qk(0)
            issue_qk(1)
            acc_of = {}
            ac = [0]
            for idx, (hi, kt) in enumerate(steps):
                h = heads[hi]
                if idx % 2 == 0:
                    for q in (2, 3):
                        if idx + q < len(steps):
                            issue_qk(idx + q)
                if kt == 0:
                    acc_of[hi] = accs[ac[0] % len(accs)]
                    ac[0] += 1
                a_, ab_ = acc_of[hi]
                t_, b_ = sbuf_of.pop(idx)
                jp = nxt("pt", 3)
                act(PT[:, jp, :], t_[:, :], AF.Exp, [b_], [BPT[jp], b_], scale=0.125)
                mm(a_, h["v_fn"](kt), PT[:, jp, :], kt == 0, kt == h["nkt"] - 1, [BPT[jp]] + list(h["rd_v"]), [ab_])
                if kt == h["nkt"] - 1:
                    rows, orow = h["rows"], h["orow"]
                    recip(rec_ap[orow, :], a_[orow, :], [ab_], [Brec, ab_])
                    tt(h["out_ap"], a_[rows, :], rec_ap[orow, :], ALU.mult, [ab_, Brec], list(h["wr_out"]) + [ab_])

        def layer_norm_tile(i, lng, lnb, sc, Bln, Bsc):
            k = nxt("st", 4)
            s = stt_[:, k, :]
            B_ = Bst[k]
            red(s[:, 0:1], X[:, i, :], ALU.add, [Bx[i]], [B_])
            act(sc, X[:, i, :], AF.Square, [Bx[i]], [Bsc])
            red(s[:, 1:2], sc, ALU.add, [Bsc], [B_])
            ts(s[:, 2:3], s[:, 0:1], 1.0 / D, None, ALU.mult, None, [B_], [B_])
            tt(s[:, 3:4], s[:, 2:3], s[:, 2:3], ALU.mult, [B_], [B_])
            stt(s[:, 4:5], s[:, 1:2], 1.0 / D, s[:, 3:4], ALU.mult, ALU.subtract, [B_], [B_])
            ts(s[:, 4:5], s[:, 4:5], EPS, None, ALU.add, None, [B_], [B_])
            act(s[:, 5:6], s[:, 4:5], AF.Sqrt, [B_], [B_])
            recip(s[:, 6:7], s[:, 5:6], [B_], [B_])
            stt(s[:, 7:8], s[:, 2:3], -1.0, s[:, 6:7], ALU.mult, ALU.mult, [B_], [B_])
            act(sc, X[:, i, :], AF.Identity, [Bx[i], B_], [Bsc], scale=s[:, 6:7], bias=s[:, 7:8])
            tt(sc, sc, lng, ALU.mult, [Bsc, Bln], [Bsc])
            tt(X[:, i, :], sc, lnb, ALU.add, [Bsc, Bln], [Bx[i]])

        def ln_batched(prep_fn, post_fn, lng, lnb, Bln):
            nonlocal live_FA
            Bsc = new_bufs(ln_prev_FA[0], ["lnsc0", "lnsc1"])
            live_FA = live_FA + Bsc
            scs = [FA[:, 2048:3072], FA[:, 3072:4096]]
            s = stt_[:]
            for i in range(NT):
                prep_fn(i)
                k = i % 2
                red(s[:, 0, i:i + 1], X[:, i, :], ALU.add, [Bx[i]], [Bst[0]])
                P.op("act", lambda e, i=i, k=k: e.activation(out=scs[k], in_=X[:, i, :], func=AF.Square, accum_out=s[:, 1, i:i + 1]),
                     [Bx[i]], [Bsc[k], Bst[1]])
            ts(s[:, 0, :], s[:, 0, :], 1.0 / D, None, ALU.mult, None, Bst, Bst)
            tt(s[:, 2, :], s[:, 0, :], s[:, 0, :], ALU.mult, Bst, Bst)
            stt(s[:, 1, :], s[:, 1, :], 1.0 / D, s[:, 2, :], ALU.mult, ALU.subtract, Bst, Bst)
            ts(s[:, 1, :], s[:, 1, :], EPS, None, ALU.add, None, Bst, Bst)
            act(s[:, 1, :], s[:, 1, :], AF.Sqrt, Bst, Bst)
            recip(s[:, 1, :], s[:, 1, :], Bst, Bst)
            stt(s[:, 3, :], s[:, 0, :], -1.0, s[:, 1, :], ALU.mult, ALU.mult, Bst, Bst)
            for i in range(NT + 1):
                if i < NT:
                    k = i % 2
                    act(scs[k], X[:, i, :], AF.Identity, [Bx[i]] + Bst, [Bsc[k]], scale=s[:, 1, i:i + 1], bias=s[:, 3, i:i + 1])
                    tt(scs[k], scs[k], lng, ALU.mult, [Bsc[k], Bln], [Bsc[k]], eng="pool")
                    tt(X[:, i, :], scs[k], lnb, ALU.add, [Bsc[k], Bln], [Bx[i]])
                if i >= 1:
                    post_fn(i - 1)

        ln_prev_FA = [[]]

        def load_ln(li, which):
            nonlocal live_FA
            ln_prev_FA[0] = list(live_FA)
            Bn = new_bufs(live_FA, ["ln", "lnsc"])
            live_FA = Bn
            r0 = li * 4 + which * 2
            dma("sp", FA[:, 0:1024], bass.AP(lnv_h, r0 * D, [[0, 128], [1, D]]), [], [Bn[0]], "fa")
            dma("sp", FA[:, 1024:2048], bass.AP(lnv_h, (r0 + 1) * D, [[0, 128], [1, D]]), [], [Bn[0]], "fa")
            return FA[:, 0:1024], FA[:, 1024:2048], FA[:, 2048:3072], Bn[0], Bn[1]

        def out_proj_ln(li, nch, wout_h, OT_fn, B_OT, last_for_next):
            nonlocal live_AR
            Bw = new_bufs([], ["wout"])
            wo = ARv[:, 28672:28672 + nch * 1024].rearrange("p (c f) -> p c f", c=nch)
            P.handoff(live_AR + live_AR_tail[0], Bw)
            live_AR_tail[0] = Bw
            dma("pool", wo, wout_h.ap().rearrange("(c p) f -> p c f", p=128), [], Bw, "wout")
            lng, lnb, sc, Bln, Bsc = load_ln(li, 0)
            def opA(i):
                for half in range(2):
                    j = nxt("p", 2)
                    for p in range(nch):
                        mm(ps_p[j][:, :], OT_fn(p, i), wo[:, p, half * 512:(half + 1) * 512], p == 0, p == nch - 1,
                           list(B_OT(p, i)) + Bw, [Bps_p[j]])
                    stt(X[:, i, half * 512:(half + 1) * 512], X[:, i, half * 512:(half + 1) * 512], ALPHA, ps_p[j][:, :],
                        ALU.mult, ALU.add, [Bx[i], Bps_p[j]], [Bx[i], Bps_p[j]])

            ln_batched(opA, make_xT, lng, lnb, Bln)

        live_AR_tail = [[]]

        def moe(li):
            nonlocal live_AR, live_FA
            for i in range(NT):
                for c in range(8):
                    mm(ps_m[:, i * 16:(i + 1) * 16], XT[:, c, i * 128:(i + 1) * 128], rwb[:, c, :], c == 0, c == 7,
                       [BxT[i], Brw], [Bps_m])
            AFF = RT[:, 0, :]; SEL = RT[:, 1, :]; T2 = RT[:, 2, :]; T3 = RT[:, 3, :]; SM = RT[:, 4, :]
            M1 = SM[:, 0:64]; M2 = SM[:, 64:128]; GS = SM[:, 128:192]; GM = SM[:, 192:208]; DEN = SM[:, 208:224]
            RB_ = [BRT]
            act(AFF, ps_m[:, 0:256], AF.Sigmoid, [Bps_m], [BRT, Bps_m])
            tt(SEL.rearrange("p (t e) -> p t e", e=16), AFF.rearrange("p (t e) -> p t e", e=16),
               bass.AP(rbt, 0, [[16, 128], [0, 16], [1, 16]]), ALU.add, [BRT, Brb], RB_)
            sel3 = SEL.rearrange("p (g e) -> p g e", e=4)
            red(M1, sel3, ALU.max, RB_, RB_)
            tt(T2.rearrange("p (g e) -> p g e", e=4), sel3, M1.to_broadcast([128, 64, 4]), ALU.is_equal, RB_, RB_)
            stt(T3, T2, -1.0e9, SEL, ALU.mult, ALU.add, RB_, RB_)
            red(M2, T3.rearrange("p (g e) -> p g e", e=4), ALU.max, RB_, RB_)
            tt(GS, M1, M2, ALU.add, RB_, RB_)
            red(GM, GS.rearrange("p (t g) -> p t g", g=4), ALU.max, RB_, RB_)
            tt(GS.rearrange("p (t g) -> p t g", g=4), GS.rearrange("p (t g) -> p t g", g=4), GM.to_broadcast([128, 16, 4]),
               ALU.is_equal, RB_, RB_)
            tt(T2.rearrange("p (g e) -> p g e", e=4), sel3, M2.to_broadcast([128, 64, 4]), ALU.is_ge, RB_, RB_)
            tt(T2.rearrange("p (g e) -> p g e", e=4), T2.rearrange("p (g e) -> p g e", e=4), GS.to_broadcast([128, 64, 4]),
               ALU.mult, RB_, RB_)
            tt(T3, T2, AFF, ALU.mult, RB_, RB_)
            red(DEN, T3.rearrange("p (t e) -> p t e", e=16), ALU.add, RB_, RB_)
            recip(DEN, DEN, RB_, RB_)
            tt(GATES[:].rearrange("p (t e) -> p t e", e=16), T3.rearrange("p (t e) -> p t e", e=16),
               DEN.to_broadcast([128, 16, 16]), ALU.mult, RB_, [Bgates])
            BRT2 = new_bufs(live_FA, ["RT2"])[0]
            live_FA = [BRT2]
            RT2 = FA[:, 0:2048].rearrange("p (a b) -> p a b", a=8)
            R2_ = [BRT2]
            MASK = T2
            cp(maskbf[:], MASK, RB_, [Bmaskbf])
            mm(ps_m[:, 0:256], ltri[:], maskbf[:], True, True, [Bltri, Bmaskbf], [Bps_m])
            mm(ps_m[:, 256:512], onesm[:], maskbf[:], True, True, [Bones, Bmaskbf], [Bps_m])
            PRE = RT2[:, 0, :]; ISZ = RT2[:, 1, :]; FIRST = RT2[:, 2, :]; SECOND = RT2[:, 3, :]; POS = RT2[:, 4, :]
            TMP = RT2[:, 5, :]; OFFS = RT2[:, 6, :]; TTs = RT2[:, 7, :]
            cp(POS, ps_m[:, 0:256], [Bps_m], R2_ + [Bps_m])
            cp(TTs, ps_m[:, 256:512], [Bps_m], R2_ + [Bps_m])
            offs3 = OFFS.rearrange("p (t e) -> p t e", e=16)
            tts3 = TTs.rearrange("p (t e) -> p t e", e=16)
            memset(offs3[:, 0, :], 0.0, R2_)
            for i in range(1, NT):
                tt(offs3[:, i, :], offs3[:, i - 1, :], tts3[:, i - 1, :], ALU.add, R2_, R2_)
            TOTE = RT[:, 5, 0:16]
            tt(TOTE, offs3[:, NT - 1, :], tts3[:, NT - 1, :], ALU.add, R2_, RB_)
            red(RT[:, 5, 16:17], TOTE, ALU.max, RB_, RB_)
            ts(RT[:, 5, 17:18], RT[:, 5, 16:17], float(CAP) + 0.5, None, ALU.is_gt, None, RB_, RB_)
            cp(fi[:, li:li + 1], RT[:, 5, 17:18], RB_, [Bfi[li]])
            for e_ in ENGS:
                P.op(e_, lambda e, r=regs[e_]: e.reg_load(r, fi[0:1, li:li + 1]), [Bfi[li]], [])
            tt(POS, POS, OFFS, ALU.add, R2_, R2_)
            tt(POS.rearrange("p (t e) -> p t e", e=16), POS.rearrange("p (t e) -> p t e", e=16),
               bass.AP(ecap, 0, [[16, 128], [0, 16], [1, 16]]), ALU.add, R2_ + [Becap], R2_)
            m3 = MASK.rearrange("p (g e) -> p g e", e=4)
            pre3 = PRE.rearrange("p (g e) -> p g e", e=4)
            memset(PRE, 0.0, R2_)
            cp(pre3[:, :, 1], m3[:, :, 0], RB_ + R2_, R2_)
            tt(pre3[:, :, 2], m3[:, :, 0], m3[:, :, 1], ALU.add, RB_ + R2_, R2_)
            tt(pre3[:, :, 3], pre3[:, :, 2], m3[:, :, 2], ALU.add, RB_ + R2_, R2_)
            ts(ISZ, PRE, 0.0, None, ALU.is_equal, None, R2_, R2_)
            tt(FIRST, MASK, ISZ, ALU.mult, RB_ + R2_, R2_)
            tt(SECOND, MASK, FIRST, ALU.subtract, RB_ + R2_, R2_)
            for k_, (sel_, val_, bufs_) in enumerate(((FIRST, POS, R2_), (SECOND, POS, R2_), (FIRST, GATES[:], R2_ + [Bgates]),
                                                      (SECOND, GATES[:], R2_ + [Bgates]))):
                tt(TMP, sel_, val_, ALU.mult, bufs_, R2_)
                red(SLF[:, k_, :], TMP.rearrange("p (t e) -> p t e", e=16), ALU.add, R2_, [Bslf])
            cp(SLI[:], SLF[:, 0:2, :], [Bslf], [Bsli])

            P.region_begin(("R", li))
            names = []
            for s_ in range(2):
                names += ["rwg%d" % s_, "rwu%d" % s_, "rwd%d" % s_]
            names += ["xstm0", "xstm1", "xsT0", "xsT1"]
            Bw = new_bufs(live_AR + live_AR_tail[0], names)
            live_AR = Bw
            live_AR_tail[0] = []

            def load_expert_w(e_):
                s_ = e_ % 2
                base = s_ * 12288
                Wg = ARv[:, base:base + 4096].rearrange("p (c f) -> p c f", c=8)
                Wu = ARv[:, base + 4096:base + 8192].rearrange("p (c f) -> p c f", c=8)
                Wd = ARv[:, base + 8192:base + 12288].rearrange("p (c f) -> p c f", c=4)
                dma("sp", Wg, wbg_h.ap()[li, e_].rearrange("(c p) f -> p c f", p=128), [BWB[li]], [Bw[3 * s_]], "rwg%d" % s_)
                dma("sp", Wu, wbu_h.ap()[li, e_].rearrange("(c p) f -> p c f", p=128), [BWB[li]], [Bw[3 * s_ + 1]], "rwu%d" % s_)
                dma("sp", Wd, wbd_h.ap()[li, e_].rearrange("(c p) f -> p c f", p=128), [BWB[li]], [Bw[3 * s_ + 2]], "rwd%d" % s_)

            load_expert_w(0)
            load_expert_w(1)
            Bxb = new_bufs(live_FA, ["xb%d" % q for q in range(8)])
            live_FA = Bxb
            xb16 = FA[:].bitcast(BF16).rearrange("p (k f) -> p k f", k=8)
            for i in range(NT):
                kq = i % 8
                cp(xb16[:, kq, :], X[:, i, :], [Bx[i]], [Bxb[kq]], eng=("act" if i % 2 == 0 else "dve"))
                for k_ in range(2):
                    P.dma("pool", lambda e, i=i, k_=k_, kq=kq: e.indirect_dma_start(
                        out=xs_h.ap(), out_offset=bass.IndirectOffsetOnAxis(ap=SLI[:, k_, i:i + 1], axis=0),
                        in_=xb16[:, kq, :], in_offset=None),
                        [Bxb[kq], Bsli], [BXS], "rsc%d" % kq)
            P.region_end()
            for i in range(NT):
                P.op("act", lambda e, i=i: e.mul(out=X[:, i, :], in_=X[:, i, :], mul=ALPHA), [Bx[i]], [Bx[i]])

            P.region_begin(("R", li))
            Bf = new_bufs(live_FA, ["ys0", "ys1", "hTr", "yb2"])
            live_FA = Bf
            Bys = Bf[0:2]; BhTr = Bf[2]
            ysb = [FA[:, 0:1024], FA[:, 1024:2048]]
            hTr = FA[:, 2048:2816].bitcast(BF16).rearrange("p (c t) -> p c t", c=4)
            nst = CAP // 128
            xstm = [ARv[:, 24576 + k * 3072:24576 + (k + 1) * 3072].rearrange("p (s f) -> p s f", s=nst) for k in range(2)]
            xsT = [ARv[:, 30720 + k * 3072:30720 + (k + 1) * 3072].rearrange("p (c t) -> p c t", c=8) for k in range(2)]
            hTrs = [FA[:, 2048 + k * 768:2816 + k * 768].bitcast(BF16).rearrange("p (c t) -> p c t", c=4) for k in range(2)]
            BhTrs = [Bf[2], Bf[3]]

            def wviews(e_):
                s_ = e_ % 2
                base = s_ * 12288
                return (ARv[:, base:base + 4096].rearrange("p (c f) -> p c f", c=8),
                        ARv[:, base + 4096:base + 8192].rearrange("p (c f) -> p c f", c=8),
                        ARv[:, base + 8192:base + 12288].rearrange("p (c f) -> p c f", c=4))

            def ex_load_x(e_):
                s_ = e_ % 2
                dma("sp", xstm[s_], xs_h.ap()[e_ * CAP:(e_ + 1) * CAP, :].rearrange("(s p) f -> p s f", p=128), [BXS], [Bw[6 + s_]], "rxs%d" % s_)

            def ex_A(e_):
                s_ = e_ % 2
                Bxm, BxT_ = Bw[6 + s_], Bw[8 + s_]
                for st_ in range(nst):
                    for c in range(8):
                        tr(ps_t[:, c * 128:(c + 1) * 128], xstm[s_][:, st_, c * 128:(c + 1) * 128], [Bxm], [Bps_t])
                    cp(xsT[s_][:, :, st_ * 128:(st_ + 1) * 128], ps_t[:].rearrange("p (c t) -> p c t", c=8), [Bps_t], [BxT_, Bps_t],
                       eng=("dve" if st_ % 2 == 0 else "act"))

            def ex_B(e_):
                s_ = e_ % 2
                Wg, Wu, Wd = wviews(e_)
                Bg, Bu = Bw[3 * s_], Bw[3 * s_ + 1]
                BxT_ = Bw[8 + s_]
                hT_, BhT_ = hTrs[s_], BhTrs[s_]
                for fc in range(4):
                    jg = nxt("s", 2)
                    ju = nxt("a", 2)
                    for c in range(8):
                        mm(ps_s[jg][:, 0:CAP], Wg[:, c, fc * 128:(fc + 1) * 128], xsT[s_][:, c, :], c == 0, c == 7, [BxT_, Bg], [Bps_s[jg]])
                    for c in range(8):
                        mm(ps_a[ju][:, 0:CAP], Wu[:, c, fc * 128:(fc + 1) * 128], xsT[s_][:, c, :], c == 0, c == 7, [BxT_, Bu], [Bps_a[ju]])
                    jp = nxt("pt", 3)
                    act(PT[:, jp, 0:CAP], ps_s[jg][:, 0:CAP], AF.Silu, [Bps_s[jg]], [BPT[jp], Bps_s[jg]])
                    tt(hT_[:, fc, :], ps_a[ju][:, 0:CAP], PT[:, jp, 0:CAP], ALU.mult, [Bps_a[ju], BPT[jp]], [BhT_, Bps_a[ju]])

            def ex_C(e_):
                s_ = e_ % 2
                Wg, Wu, Wd = wviews(e_)
                Bd = Bw[3 * s_ + 2]
                hT_, BhT_ = hTrs[s_], BhTrs[s_]
                for st_ in range(nst):
                    ky = nxt("ys", 2)
                    for half in range(2):
                        j = nxt("p", 2)
                        for fc in range(4):
                            mm(ps_p[j][:, :], hT_[:, fc, st_ * 128:(st_ + 1) * 128], Wd[:, fc, half * 512:(half + 1) * 512], fc == 0, fc == 3,
                               [BhT_, Bd], [Bps_p[j]])
                        cp(ysb[ky][:, half * 512:(half + 1) * 512], ps_p[j][:, :], [Bps_p[j]], [Bys[ky], Bps_p[j]],
                           eng=("act" if half == 0 else "dve"))
                    r0 = e_ * CAP + st_ * 128
                    dma("sp", ys_h.ap()[r0:r0 + 128, :], ysb[ky], [Bys[ky]], [BYS], "rys%d" % ky)

            ex_load_x(0)
            ex_A(0)
            for e_ in range(NE):
                if e_ + 1 < NE:
                    ex_load_x(e_ + 1)
                    if e_ + 1 >= 2:
                        load_expert_w(e_ + 1)
                ex_B(e_)
                if e_ + 1 < NE:
                    ex_A(e_ + 1)
                ex_C(e_)
            P.handoff([Bf[2], Bf[3]], [Bf[2], Bf[3]])
            Byb8 = new_bufs(Bw, ["yb8_%d" % q for q in range(8)])
            live_AR = Bw + Byb8
            yb8 = ARv[:, 0:16384].bitcast(F32).rearrange("p (k f) -> p k f", k=8)
            ybs = [(yb8[:, q, :], Byb8[q]) for q in range(8)]
            for i in range(NT):
                for k_ in range(2):
                    kb_ = nxt("yb", 8)
                    yb, Byb = ybs[kb_]
                    P.dma("pool", lambda e, i=i, k_=k_, yb=yb: e.indirect_dma_start(
                        out=yb, out_offset=None, in_=ys_h.ap(),
                        in_offset=bass.IndirectOffsetOnAxis(ap=SLI[:, k_, i:i + 1], axis=0)),
                        [BYS, Bsli], [Byb], "rg%d" % kb_)
                    stt(X[:, i, :], yb, SLF[:, 2 + k_, i:i + 1], X[:, i, :], ALU.mult, ALU.add, [Byb, Bslf, Bx[i]], [Bx[i]])
            P.region_end()

            P.region_begin(("D", li))
            names = []
            for s_ in range(2):
                names += ["wg%d" % s_, "wu%d" % s_, "wd%d" % s_]
            Bw = new_bufs(live_AR + live_AR_tail[0], names + ["hT"])
            live_AR = Bw
            live_AR_tail[0] = []
            BhT = Bw[6]
            hT = ARv[:, 24576:32768].rearrange("p (c t) -> p c t", c=4)
            for e_ in range(NE):
                s_ = e_ % 2
                base = s_ * 12288
                Wg = ARv[:, base:base + 4096].rearrange("p (c f) -> p c f", c=8)
                Wu = ARv[:, base + 4096:base + 8192].rearrange("p (c f) -> p c f", c=8)
                Wd = ARv[:, base + 8192:base + 12288].rearrange("p (c f) -> p c f", c=4)
                Bg, Bu, Bd = Bw[3 * s_], Bw[3 * s_ + 1], Bw[3 * s_ + 2]
                dma("pool", Wg, wg_h.ap()[li, e_].rearrange("(c p) f -> p c f", p=128), [], [Bg], "wg%d" % s_)
                dma("pool", Wu, wu_h.ap()[li, e_].rearrange("(c p) f -> p c f", p=128), [], [Bu], "wu%d" % s_)
                dma("pool", Wd, wd_h.ap()[li, e_].rearrange("(c p) f -> p c f", p=128), [], [Bd], "wd%d" % s_)
                for tb in range(4):
                    rdx = [BxT[4 * tb + q] for q in range(4)]
                    for fc in range(4):
                        jg = nxt("s", 2)
                        ju = nxt("a", 2)
                        for c in range(8):
                            mm(ps_s[jg][:, :], Wg[:, c, fc * 128:(fc + 1) * 128], XT[:, c, tb * 512:(tb + 1) * 512], c == 0, c == 7,
                               rdx + [Bg], [Bps_s[jg]])
                        for c in range(8):
                            mm(ps_a[ju][:, :], Wu[:, c, fc * 128:(fc + 1) * 128], XT[:, c, tb * 512:(tb + 1) * 512], c == 0, c == 7,
                               rdx + [Bu], [Bps_a[ju]])
                        jp = nxt("pt", 3)
                        act(PT[:, jp, :], ps_s[jg][:, :], AF.Silu, [Bps_s[jg]], [BPT[jp], Bps_s[jg]])
                        tt(hT[:, fc, tb * 512:(tb + 1) * 512], ps_a[ju][:, :], PT[:, jp, :], ALU.mult, [Bps_a[ju], BPT[jp]],
                           [BhT, Bps_a[ju]])
                for i in range(NT):
                    for half in range(2):
                        j = nxt("p", 2)
                        for fc in range(4):
                            mm(ps_p[j][:, :], hT[:, fc, i * 128:(i + 1) * 128], Wd[:, fc, half * 512:(half + 1) * 512], fc == 0, fc == 3,
                               [BhT, Bd], [Bps_p[j]])
                        stt(X[:, i, half * 512:(half + 1) * 512], ps_p[j][:, :], GATES[:, i * 16 + e_:i * 16 + e_ + 1],
                            X[:, i, half * 512:(half + 1) * 512], ALU.mult, ALU.add, [Bps_p[j], Bgates, Bx[i]], [Bx[i], Bps_p[j]])
            P.region_end()
            lng, lnb, sc, Bln, Bsc = load_ln(li, 1)

            def post2(i):
                if li == DEPTH - 1:
                    dma("sp", y_h.ap()[i * 128:(i + 1) * 128, :], X[:, i, :], [Bx[i]], [Bydram], "x%d" % i)
                else:
                    make_xT(i)

            ln_batched(lambda i: None, post2, lng, lnb, Bln)

        names = ["QT%d_%d" % (p, t) for p in range(8) for t in range(4)] + ["KT%d" % t for t in range(NT)] + \
                ["V%d" % t for t in range(NT)] + ["w0", "w1"]
        Bn = new_bufs(live_AR, names)
        live_AR = Bn[:64]
        live_AR_tail[0] = Bn[64:]
        BQT = [[Bn[p * 4 + t] for t in range(4)] for p in range(8)]
        BKT = Bn[32:48]; BV = Bn[48:64]; BW = Bn[64:66]
        QT = ARv[:, 0:16384].rearrange("p (c t) -> p c t", c=8)
        KT = ARv[:, 16384:20480].rearrange("p (c t) -> p c t", c=2)
        VA = ARv[:, 20480:28672].rearrange("p (t h d) -> p t h d", t=16, h=4)
        WS = [ARv[:, 28672 + s_ * 4096:28672 + (s_ + 1) * 4096].rearrange("p (c f) -> p c f", c=8) for s_ in range(2)]
        memset(ARv[:, 20480:28672], 1.0, BV, eng="pool")
        BFn = new_bufs(live_FA, ["G", "S1", "S2", "S3"])
        live_FA = BFn
        BG, BS1, BS2, BS3 = BFn
        G0 = FA[:, 0:512]; G1 = FA[:, 512:1024]; S1 = FA[:, 1024:1536]; S2 = FA[:, 1536:2048]; S3 = FA[:, 2048:2560]
        dma("sp", G0.rearrange("p (h d) -> p h d", h=8), bass.AP(aqn_h, 0, [[0, 128], [0, 8], [1, 64]]), [], [BG], "fa")
        dma("sp", G1[:, 0:256].rearrange("p (h d) -> p h d", h=4), bass.AP(aqn_h, 0, [[0, 128], [0, 4], [1, 64]]), [], [BG], "fa")
        dma("sp", G1[:, 256:512].rearrange("p (h d) -> p h d", h=4), bass.AP(akn_h, 0, [[0, 128], [0, 4], [1, 64]]), [], [BG], "fa")

        BSb = new_bufs(live_FA, ["S1b", "S2b", "S3b"])
        live_FA = live_FA + BSb
        SSETS = [(1024, 1536, 2048, BS1, BS2, BS3), (2560, 3072, 3584, BSb[0], BSb[1], BSb[2])]
        cnt["sset"] = 0

        def rms_rope1(j, i, G, kcs):
            k = nxt("st", 4)
            sset = SSETS[nxt("sset", 2)]
            o1, o2, o3, B1, B2, B3 = sset
            s1 = FA[:, o1:o1 + 512]
            ss = stt_[:, k, 0:8]; B_ = Bst[k]
            act(s1, ps_p[j][:, :], AF.Square, [Bps_p[j]], [B1, Bps_p[j]])
            red(ss, s1.rearrange("p (h d) -> p h d", h=8), ALU.add, [B1], [B_])
            ts(ss, ss, 1.0 / 64, EPS, ALU.mult, ALU.add, [B_], [B_])
            act(ss, ss, AF.Sqrt, [B_], [B_])
            recip(ss, ss, [B_], [B_])
            tt(s1.rearrange("p (h d) -> p h d", h=8), ps_p[j][:, :].rearrange("p (h d) -> p h d", h=8),
               ss.to_broadcast([128, 8, 64]), ALU.mult, [Bps_p[j], B_], [B1, Bps_p[j]])
            tt(s1, s1, G, ALU.mult, [B1, BG], [B1], eng="pool")
            return (sset, kcs)

        def rms_rope2(state):
            (o1, o2, o3, B1, B2, B3), kcs = state
            s1 = FA[:, o1:o1 + 512]; s2 = FA[:, o2:o2 + 512]; s3 = FA[:, o3:o3 + 512]
            tt(s2.rearrange("p (h d) -> p h d", h=8), s1.rearrange("p (h d) -> p h d", h=8),
               bass.AP(cs, kcs * 128, [[256, 128], [0, 8], [1, 64]]), ALU.mult, [B1, Bcs[kcs]], [B2])
            for hf in range(2):
                o = bass.AP(FA, o3 + hf * 16, [[4096, 128], [64, 8], [32, 2], [1, 16]])
                a = bass.AP(FA, o1 + (1 - hf) * 16, [[4096, 128], [64, 8], [32, 2], [1, 16]])
                b = bass.AP(cs, kcs * 128 + 64 + hf * 16, [[256, 128], [0, 8], [32, 2], [1, 16]])
                tt(o, a, b, ALU.mult, [B1, Bcs[kcs]], [B3], eng="pool")
            km = nxt("xq", 4)
            src = (xbf[:, km // 2, (km % 2) * 512:(km % 2) * 512 + 512], Bxq[km])
            tt(src[0], s2, s3, ALU.add, [B2, B3], [Bxq[km]])
            return src

        def tr_pairs(src, npair, src_off, dst_fn, wr_fn):
            sap, sbuf_ = src
            for pr in range(npair):
                tr(ps_t[:, pr * 128:(pr + 1) * 128], sap[:, src_off + pr * 128:src_off + (pr + 1) * 128], [sbuf_], [Bps_t])
            for pr in range(npair):
                cp(dst_fn(pr), ps_t[:, pr * 128:(pr + 1) * 128], [Bps_t], list(wr_fn(pr)) + [Bps_t], eng="act")

        def load_ws(cb):
            s_ = cb % 2
            dma("pool", WS[s_], awin_h.ap()[:, cb * 512:(cb + 1) * 512].rearrange("(c p) f -> p c f", p=128), [], [BW[s_]], "w%d" % s_)

        pend = []
        half2 = []

        def l0A(cb, i):
            s_ = cb % 2
            tsl = slice(i * 128, (i + 1) * 128)
            j = nxt("p", 2)
            for c in range(8):
                mm(ps_p[j][:, :], XT[:, c, tsl], WS[s_][:, c, :], c == 0, c == 7, [BxT[i], BW[s_]], [Bps_p[j]])
            if cb < 2:
                kcs = nxt("cs", 2)
                dma("sp", cs[:, kcs, 0:64], cosA_h.ap()[tsl, :], [], [Bcs[kcs]], "cs%d" % kcs)
                dma("sp", cs[:, kcs, 64:128], sinA_h.ap()[tsl, :], [], [Bcs[kcs]], "cs%d" % kcs)
                st_ = rms_rope1(j, i, G0 if cb == 0 else G1, kcs)

                def second(st_=st_, cb=cb, i=i, tsl=tsl):
                    kb = rms_rope2(st_)
                    if cb == 0:
                        pend.append(lambda: tr_pairs(kb, 4, 0, lambda pr: QT[:, pr, tsl], lambda pr: [BQT[pr][i // 4]]))
                    else:
                        def f():
                            tr_pairs(kb, 2, 0, lambda pr: QT[:, 4 + pr, tsl], lambda pr: [BQT[4 + pr][i // 4]])
                            tr_pairs(kb, 2, 256, lambda pr: KT[:, pr, tsl], lambda pr: [BKT[i]])
                        pend.append(f)
                half2.append(second)
            else:
                cp(VA[:, i, 0:2, 0:64], ps_p[j][:, 0:128].rearrange("p (h d) -> p h d", h=2), [Bps_p[j]], [BV[i], Bps_p[j]])
                cp(VA[:, i, 2:4, 64:128], ps_p[j][:, 128:256].rearrange("p (h d) -> p h d", h=2), [Bps_p[j]], [BV[i], Bps_p[j]])
                km = nxt("xq", 4)
                kb = (xbf[:, km // 2, (km % 2) * 512:(km % 2) * 512 + 512], Bxq[km])
                cp(kb[0][:, 0:256], ps_p[j][:, 256:512], [Bps_p[j]], [Bxq[km], Bps_p[j]], eng="act")
                pend.append(lambda: tr_pairs(kb, 2, 0, lambda pr: QT[:, 6 + pr, tsl], lambda pr: [BQT[6 + pr][i // 4]]))

        cnt["cs"] = 0
        cnt["xq"] = 0
        Bxq = new_bufs(Bxbf, ["xq%d" % q for q in range(4)])
        load_ws(0)
        load_ws(1)
        for cb in range(3):
            if cb == 1:
                pass
            for i in range(NT):
                if cb == 1 and i == 0:
                    load_ws(2)
                if len(pend) >= 2:
                    pend.pop(0)()
                prev2 = half2.pop(0) if half2 else None
                l0A(cb, i)
                if prev2 is not None:
                    prev2()
        while half2:
            half2.pop(0)()
        while pend:
            pend.pop(0)()
        P.handoff(Bxq + Bxbf, Bxbf)

        rec = FA[:, 2560:3072]
        Brec = new_bufs(BSb, ["rec"])[0]
        heads = []
        for p in range(8):
            for tb in range(4):
                for half in range(2):
                    rows = slice(0, 64) if half == 0 else slice(64, 128)
                    orow = slice(64, 128) if half == 0 else slice(0, 64)
                    qsl = slice(tb * 512, (tb + 1) * 512)
                    kp = p // 3 if p < 6 else p - 6
                    hv = kp if half == 0 else 2 + kp
                    if p < 6:
                        heads.append(dict(q_ap=QT[rows, p, qsl], k_fn=(lambda kt, rows=rows, kp=kp: KT[rows, kp, kt * 128:(kt + 1) * 128]),
                                          v_fn=(lambda kt, hv=hv: VA[:, kt, hv, :]), nkt=16, out_ap=XT[rows, p, qsl], rows=rows, orow=orow,
                                          rd_q=[BQT[p][tb]], rd_k=BKT, rd_v=BV, wr_out=[BxT[4 * tb + q] for q in range(4)]))
                    else:
                        heads.append(dict(q_ap=QT[rows, p, qsl], k_fn=(lambda kt, rows=rows, kp=kp: memKT[rows, kp, kt * 128:(kt + 1) * 128]),
                                          v_fn=(lambda kt, hv=hv: memV[:, kt, hv, :]), nkt=2, out_ap=XT[rows, p, qsl], rows=rows, orow=orow,
                                          rd_q=[BQT[p][tb]], rd_k=[BmemKT], rd_v=[BmemV], wr_out=[BxT[4 * tb + q] for q in range(4)]))
        ring4 = [(ps_s[0], Bps_s[0]), (ps_s[1], Bps_s[1]), (ps_p[0], Bps_p[0]), (ps_p[1], Bps_p[1])]
        ring2 = [(ps_s[0], Bps_s[0]), (ps_s[1], Bps_s[1])]
        memset(FA[:, 1024:2048], 0.0, [BS1, BS2])
        zero_fill()
        precast_weights(0)
        accs4 = [(ps_a[0][:, :], Bps_a[0]), (ps_a[1][:, :], Bps_a[1]), (ps_m[:, :], Bps_m), (ps_t[:].bitcast(F32), Bps_t)]
        run_attn(heads, rec, Brec, ring4, 2, accs4)
        live_FA = live_FA + [Brec]
        out_proj_ln(0, 8, awout_h, lambda p, i: XT[:, p, i * 128:(i + 1) * 128], lambda p, i: [BxT[i]], True)
        moe(0)

        names = ["OT%d_%d" % (p, t) for p in range(6) for t in range(NT)] + ["uw0", "uw1", "QTu0", "KTu0", "Vu0", "QTu1", "KTu1", "Vu1"]
        Bn = new_bufs(live_AR + live_AR_tail[0], names)
        live_AR = Bn
        live_AR_tail[0] = []
        BOT = [[Bn[p * NT + t] for t in range(NT)] for p in range(6)]
        BUW = Bn[96:98]
        BQTu = [Bn[98], Bn[101]]; BKTu = [Bn[99], Bn[102]]; BVu = [Bn[100], Bn[103]]
        OT = ARv[:, 0:12288].rearrange("p (c t) -> p c t", c=6)
        UW = [ARv[:, 12288 + s_ * 3072:12288 + (s_ + 1) * 3072].rearrange("p (c f) -> p c f", c=8) for s_ in range(2)]
        QTu = [ARv[:, 18432 + 8192 * k:20480 + 8192 * k] for k in range(2)]
        KTu = [ARv[:, 20480 + 8192 * k:22528 + 8192 * k] for k in range(2)]
        Vu = [ARv[:, 22528 + 8192 * k:26624 + 8192 * k].rearrange("p (t h d) -> p t h d", t=16, h=2) for k in range(2)]
        for k in range(2):
            memset(ARv[:, 22528 + 8192 * k:26624 + 8192 * k], 1.0, [BVu[k]], eng="pool")
        BFn = new_bufs(live_FA, ["accA", "accB"])
        live_FA = BFn
        BaccA, BaccB = BFn
        accA = FA[:, 0:2048]; accB = FA[:, 2048:4096]
        DIL = (1, 4, 16)
        units = [(pp, g) for pp in range(4) for g in range(3)]
        NU = len(units)

        def load_uw(n):
            pp, g = units[n]
            u = g * 4 + pp
            s_ = n % 2
            dma("pool", UW[s_], bwin_h.ap()[:, u * 384:(u + 1) * 384].rearrange("(c p) f -> p c f", p=128), [], [BUW[s_]], "uw%d" % s_)

        pj = {}

        def projA(n, jt):
            pp, g = units[n]
            d = DIL[g]
            tps = (S // d) // 128
            s_ = n % 2
            k = n % 2
            r = jt // tps
            l0 = (jt % tps) * 128
            kcs = nxt("tb", 2)
            dma("sp", tb16[:, kcs, :], tabB_h.ap()[g, jt * 128:(jt + 1) * 128, :], [], [Btb[kcs]], "tb%d" % kcs)
            j = nxt("p", 2)
            t0 = l0 * d + r
            rdx = [BxT[q] for q in range(t0 // 128, (t0 + 127 * d) // 128 + 1)]
            for c in range(8):
                mm(ps_p[j][:, 0:384], XT[:, c, t0:t0 + 127 * d + 1:d], UW[s_][:, c, :], c == 0, c == 7, rdx + [BUW[s_]], [Bps_p[j]])
            km = nxt("xm", 4)
            kb = km // 2
            xo = (km % 2) * 256
            Bxm_ = Bxmini[km]
            pj[(n, jt)] = km
            STG = RT[:, 4:6, :].rearrange("p a b -> p (a b)")
            cp(xbf[:, kb, xo:xo + 256], ps_p[j][:, 0:256], [Bps_p[j]], [Bxm_, Bps_p[j]], eng="act")
            cp(STG[:, 0:384], ps_p[j][:, 0:384], [Bps_p[j]], [Bstg, Bps_p[j]], eng="act")
            psv = STG[:, 0:256].rearrange("p (h d) -> p h d", h=4)
            T1 = bass.AP(tb16, kcs * 32, [[64, 128], [0, 4], [1, 16]])
            SA = RT[:, 0, 0:64].rearrange("p (h d) -> p h d", h=4)
            SB = RT[:, 1, 0:64].rearrange("p (h d) -> p h d", h=4)
            tt(SA, psv[:, :, 0:16], T1, ALU.mult, [Bstg, Btb[kcs]], [BRTa], eng="pool")
            tt(SB[:, :, 0:8], psv[:, :, 8:16],
               bass.AP(tb16, kcs * 32 + 16, [[64, 128], [0, 4], [1, 8]]), ALU.mult, [Bstg, Btb[kcs]], [BRTb], eng="pool")
            tt(SB[:, :, 8:16], psv[:, :, 0:8],
               bass.AP(tb16, kcs * 32 + 24, [[64, 128], [0, 4], [1, 8]]), ALU.mult, [Bstg, Btb[kcs]], [BRTb], eng="pool")
            tt(xbf[:, kb, xo:xo + 256].rearrange("p (h d) -> p h d", h=4)[:, :, 0:16], SA, SB, ALU.add, [BRTa, BRTb, Bxm_], [Bxm_], eng="pool")
            cp(Vu[k][:, jt, 0, 0:64], STG[:, 256:320], [Bstg], [BVu[k]], eng="pool")
            cp(Vu[k][:, jt, 1, 64:128], STG[:, 320:384], [Bstg], [BVu[k]], eng="pool")

        def projB(n, jt):
            k = n % 2
            km = pj.pop((n, jt))
            kb = km // 2
            xo = (km % 2) * 256
            tr(ps_t[:, 0:128], xbf[:, kb, xo:xo + 128], [Bxmini[km]], [Bps_t])
            tr(ps_t[:, 128:256], xbf[:, kb, xo + 128:xo + 256], [Bxmini[km]], [Bps_t])
            cp(QTu[k][:, jt * 128:(jt + 1) * 128], ps_t[:, 0:128], [Bps_t], [BQTu[k], Bps_t], eng="dve")
            cp(KTu[k][:, jt * 128:(jt + 1) * 128], ps_t[:, 128:256], [Bps_t], [BKTu[k], Bps_t], eng="dve")

        def proj_step(n, t):
            if t < NT:
                projA(n, t)
            if t >= 2:
                projB(n, t - 2)

        def kts_of(n, jt):
            pp, g = units[n]
            tps = (S // DIL[g]) // 128
            lo = (jt // tps) * tps
            hi = lo + tps - 1
            return [(jt + o, 1 + o) for o in (-1, 0, 1) if lo <= jt + o <= hi]

        qk = {}

        def attn_qk(n, jt, half):
            k = n % 2
            rows = slice(0, 64) if half == 0 else slice(64, 128)
            js = nxt("s3", 3)
            qk[(n, jt, half)] = js
            t_, b_ = ring3[js]
            for m, (kt, mi) in enumerate(kts_of(n, jt)):
                mm(t_[:, m * 128:(m + 1) * 128], KTu[k][rows, kt * 128:(kt + 1) * 128], QTu[k][rows, jt * 128:(jt + 1) * 128],
                   True, True, [BQTu[k], BKTu[k]], [b_])

        ja_cur = [0]

        ptk = {}
        PT4v = PT[:].rearrange("p a b -> p (a b)")

        def attn_pre(n, jt, half):
            kts = kts_of(n, jt)
            m0 = kts[0][1]
            nn = len(kts) * 128
            js = qk.pop((n, jt, half))
            jp = nxt("pt4", 4)
            ptk[(n, jt, half)] = jp
            pt = PT4v[:, jp * 384:jp * 384 + nn]
            t_, b_ = ring3[js]
            act(pt, t_[:, 0:nn], AF.Exp, [b_], [BPT4[jp], b_], scale=0.125)
            tt(pt, pt, maskb[:, m0 * 128:m0 * 128 + nn], ALU.mult, [BPT4[jp], Bmask], [BPT4[jp]], eng="dve")

        def attn_post(n, jt, half):
            pp, g = units[n]
            d = DIL[g]
            tps = (S // d) // 128
            k = n % 2
            kts = kts_of(n, jt)
            if half == 0:
                ja_cur[0] = nxt("a", 2)
            ja = ja_cur[0]
            jp = ptk.pop((n, jt, half))
            for m, (kt, mi) in enumerate(kts):
                mm(ps_a[ja][:, half * 128:(half + 1) * 128], Vu[k][:, kt, half, :], PT4v[:, jp * 384 + m * 128:jp * 384 + (m + 1) * 128],
                   m == 0, m == len(kts) - 1, [BPT4[jp], BVu[k]], [Bps_a[ja]])
            if half == 1:
                r = jt // tps
                l0 = (jt % tps) * 128
                t0 = l0 * d + r
                for hf, acc, Bacc in ((0, accA, BaccA), (1, accB, BaccB)):
                    dst = acc[:, t0:t0 + 127 * d + 1:d]
                    src = ps_a[ja][:, hf * 128:(hf + 1) * 128]
                    if g == 0:
                        cp(dst, src, [Bps_a[ja]], [Bacc, Bps_a[ja]])
                    else:
                        tt(dst, dst, src, ALU.add, [Bacc, Bps_a[ja]], [Bacc, Bps_a[ja]])

        def finish_pair(pp):
            recf = RT[:, 2:4, :].rearrange("p a b -> p (a b)")
            for blk in range(4):
                bs = slice(blk * 512, (blk + 1) * 512)
                for half, acc, Bacc in ((0, accA, BaccA), (1, accB, BaccB)):
                    rows = slice(0, 64) if half == 0 else slice(64, 128)
                    js = nxt("s", 2)
                    mm(ps_s[js][:, :], swpf[:], acc[:, bs], True, True, [Bacc, Bswp], [Bps_s[js]])
                    recip(recf[rows, :], ps_s[js][rows, :], [Bps_s[js]], [BRT, Bps_s[js]])
                    tt(OT[rows, pp, bs], acc[rows, bs], recf[rows, :], ALU.mult, [Bacc, BRT], BOT[pp][4 * blk:4 * blk + 4])

        cnt["tb"] = 0
        cnt["xm"] = 0
        cnt["s3"] = 0
        cnt["pt4"] = 0
        BPT4 = new_bufs(BPT, ["pt4_%d" % q for q in range(4)])
        ring3 = [(ps_s[0], Bps_s[0]), (ps_s[1], Bps_s[1]), (ps_m, Bps_m)]
        Bxmini = new_bufs(Bxbf, ["xm%d" % q for q in range(4)])
        BRTa = Buf("RTa"); BRTb = Buf("RTb"); Bstg = Buf("stg")
        P.handoff([BRT], [BRTa, BRTb, Bstg])
        load_uw(0)
        load_uw(1)
        for t in range(NT + 2):
            proj_step(0, t)
        for n in range(NU):
            pp, g = units[n]
            if n + 2 < NU:
                load_uw(n + 2)
            if n == NU - 1:
                dma("pool", UW[0][:, :, 0:256], bwin_h.ap()[:, 4608:4864].rearrange("(c p) f -> p c f", p=128), [], [BUW[0]], "uw0")
            precast_weights(1, 4 * n, 4 * n + 4)
            asteps = [(jt, half) for jt in range(NT) for half in range(2)]
            attn_qk(n, 0, 0)
            attn_qk(n, 0, 1)
            attn_pre(n, 0, 0)
            attn_pre(n, 0, 1)
            for t in range(NT + 2):
                if t < NT:
                    for half in range(2):
                        si = t * 2 + half
                        if si + 2 < len(asteps):
                            attn_qk(n, *asteps[si + 2])
                        attn_post(n, t, half)
                    if t + 1 < NT:
                        attn_pre(n, t + 1, 0)
                        attn_pre(n, t + 1, 1)
                if n + 1 < NU:
                    proj_step(n + 1, t)
            if g == 2:
                finish_pair(pp)
        P.handoff([BRTa, BRTb, Bstg, BRT], [BRT])
        P.handoff(Bxmini + Bxbf, Bxbf)
        P.handoff(BPT4 + BPT, BPT)
        ucount = NU
        BQTu0, BKTu0 = BQTu[0], BKTu[0]
        s_ = 0
        QTm = ARv[:, 18432:22528].rearrange("p (c t) -> p c t", c=2)
        Bqm = new_bufs([BQTu0, BKTu0], ["QTm%d" % t for t in range(4)])
        for i in range(NT):
            tsl = slice(i * 128, (i + 1) * 128)
            j = nxt("p", 2)
            for c in range(8):
                mm(ps_p[j][:, 0:256], XT[:, c, tsl], UW[s_][:, c, 0:256], c == 0, c == 7, [BxT[i], BUW[s_]], [Bps_p[j]])
            kb = nxt("xbf", 2)
            cp(xbf[:, kb, 0:256], ps_p[j][:, 0:256], [Bps_p[j]], [Bxbf[kb], Bps_p[j]], eng="act")
            tr_pairs((xbf[:, kb, :], Bxbf[kb]), 2, 0, lambda pr: QTm[:, pr, tsl], lambda pr: [Bqm[i // 4]])
        ring4b = [(ps_s[0], Bps_s[0]), (ps_s[1], Bps_s[1]), (ps_p[0], Bps_p[0]), (ps_p[1], Bps_p[1])]
        rec = S3
        heads = []
        for kp in range(2):
            for tb in range(4):
                for half in range(2):
                    rows = slice(0, 64) if half == 0 else slice(64, 128)
                    orow = slice(64, 128) if half == 0 else slice(0, 64)
                    qsl = slice(tb * 512, (tb + 1) * 512)
                    hv = kp if half == 0 else 2 + kp
                    heads.append(dict(q_ap=QTm[rows, kp, qsl], k_fn=(lambda kt, rows=rows, kp=kp: memKT[rows, kp, kt * 128:(kt + 1) * 128]),
                                      v_fn=(lambda kt, hv=hv: memV[:, kt, hv, :]), nkt=2, out_ap=OT[rows, 4 + kp, qsl], rows=rows, orow=orow,
                                      rd_q=[Bqm[tb]], rd_k=[BmemKT], rd_v=[BmemV], wr_out=[BOT[4 + kp][4 * tb + q] for q in range(4)]))
        run_attn(heads, rec, BaccB, ring4b, 2)
        live_AR = live_AR + Bqm
        out_proj_ln(1, 6, bwout_h, lambda p, i: OT[:, p, i * 128:(i + 1) * 128], lambda p, i: [BOT[p][i]], False)
        moe(1)
        P.finish()
        P.emit(nc, st, regs)
    return nc


def _tables():
    half = 32
    inv = 1.0 / (10000.0 ** (np.arange(0, half, 2, dtype=np.float32) / half))
    t = np.arange(S)
    row = (t // 64).astype(np.float32)[:, None] * inv[None, :]
    col = (t % 64).astype(np.float32)[:, None] * inv[None, :]
    cr, sr, cc, sc = np.cos(row), np.sin(row), np.cos(col), np.sin(col)
    cosA = np.concatenate([cr, cr, cc, cc], 1).astype(np.float32)
    sinA = np.concatenate([-sr, sr, -sc, sc], 1).astype(np.float32)
    invb = 1.0 / (500000.0 ** (np.arange(0, 16, 2, dtype=np.float32) / 16))
    tabB = np.zeros((3, S, 32), np.float32)
    for g, d in enumerate((1, 4, 16)):
        L = S // d
        i = np.arange(S)
        tok = (i % L) * d + (i // L)
        ang = tok.astype(np.float32)[:, None] * invb[None, :]
        c, s = np.cos(ang), np.sin(ang)
        tabB[g] = np.concatenate([c, c, -s, s], 1)
    k = np.arange(128)[:, None]
    q = np.arange(128)[None, :]
    m_prev = (k >= q + 64)
    m_own = (np.abs(q - k) <= 64)
    m_next = (k <= q - 64)
    masks = np.concatenate([m_prev, m_own, m_next], 1).astype(np.float32)
    return cosA, sinA, tabB, masks


_NC_CACHE = {}


def kernel(x, mem, w_mem_kv, router_w, router_b, a_w_in, a_w_out, a_q_norm, a_k_norm,
           b_w_in, b_w_out, ln1_g, ln1_b, ln2_g, ln2_b, w_gate, w_up, w_down):
    f = lambda a: np.ascontiguousarray(np.asarray(a, dtype=np.float32))
    x, mem = f(x), f(mem)
    hd = 64
    wkv = f(w_mem_kv)
    kperm = [0, 2, 1, 3]
    wkv_p = np.concatenate([wkv[:, h * hd:(h + 1) * hd] for h in kperm] + [wkv[:, 256:512]], 1)
    awin = f(a_w_in)[0]
    qc = lambda h: awin[:, h * hd:(h + 1) * hd]
    kc = lambda h: awin[:, 768 + h * hd:768 + (h + 1) * hd]
    vc = lambda h: awin[:, 1024 + h * hd:1024 + (h + 1) * hd]
    mc = lambda h: awin[:, 1280 + h * hd:1280 + (h + 1) * hd]
    cols = [qc(0), qc(6), qc(1), qc(7), qc(2), qc(8), qc(3), qc(9),
            qc(4), qc(10), qc(5), qc(11), kc(0), kc(2), kc(1), kc(3),
            vc(0), vc(1), vc(2), vc(3), mc(0), mc(2), mc(1), mc(3)]
    awin_p = np.ascontiguousarray(np.concatenate(cols, 1))
    awout = f(a_w_out)[0]
    rows = []
    for p in range(6):
        rows += [awout[p * hd:(p + 1) * hd], awout[(6 + p) * hd:(7 + p) * hd]]
    for a, b in ((0, 2), (1, 3)):
        rows += [awout[768 + a * hd:768 + (a + 1) * hd], awout[768 + b * hd:768 + (b + 1) * hd]]
    awout_p = np.ascontiguousarray(np.concatenate(rows, 0))
    bwin = f(b_w_in)[0]
    cols = []
    for g in range(3):
        for pp in range(4):
            for part in range(3):
                for h in (2 * pp, 2 * pp + 1):
                    o = part * 1536 + g * 512 + h * hd
                    cols.append(bwin[:, o:o + hd])
    for h in kperm:
        cols.append(bwin[:, 4608 + h * hd:4608 + (h + 1) * hd])
    bwin_p = np.ascontiguousarray(np.concatenate(cols, 1))
    bwout = f(b_w_out)[0]
    rows = [bwout[0:512]]
    for a, b in ((0, 2), (1, 3)):
        rows += [bwout[512 + a * hd:512 + (a + 1) * hd], bwout[512 + b * hd:512 + (b + 1) * hd]]
    bwout_p = np.ascontiguousarray(np.concatenate(rows, 0))
    lnv = np.stack([f(ln1_g)[0], f(ln1_b)[0], f(ln2_g)[0], f(ln2_b)[0],
                    f(ln1_g)[1], f(ln1_b)[1], f(ln2_g)[1], f(ln2_b)[1]], 0)
    cosA, sinA, tabB, masks = _tables()
    shared = {"wkv": wkv_p, "rw": f(router_w), "rb": f(router_b).reshape(1, 16), "awin": awin_p, "awout": awout_p,
              "aqn": f(a_q_norm).reshape(1, 64), "akn": f(a_k_norm).reshape(1, 64), "bwin": bwin_p, "bwout": bwout_p,
              "lnv": np.ascontiguousarray(lnv), "wg": f(w_gate), "wu": f(w_up), "wd": f(w_down),
              "cosA": cosA, "sinA": sinA, "tabB": tabB, "masks": masks,
              "swp": np.ascontiguousarray(np.roll(np.eye(128, dtype=np.float32), 64, axis=1)),
              "ltri": np.ascontiguousarray(np.triu(np.ones((128, 128), np.float32), 1)),
              "ecap": (np.arange(16, dtype=np.float32) * CAP).reshape(1, 16)}
    if "nc" not in _NC_CACHE:
        _NC_CACHE["nc"] = build()
    nc = _NC_CACHE["nc"]
    in_maps = []
    for b in range(8):
        m = dict(shared)
        m["x"] = x[b]
        m["mem"] = mem[b]
        in_maps.append(m)
    res = run_bass_kernel_spmd(nc, in_maps, core_ids=list(range(8)))
    return np.stack([np.asarray(r["y"], dtype=np.float32) for r in res.results], 0)
```
